# Optimizing a Trainium2 kernel written in Bass

```python
import math
import jax, jax.numpy as jnp
from jax import lax
import numpy as np

D_MODEL = 1024
BATCH = 4
SEQ = 4096
DEPTH = 1

N_ATTN_HEADS = 4
ATTN_HEAD_DIM = 64
ATTN_V_DIM = 2 * ATTN_HEAD_DIM
ATTN_QK_WIDTH = N_ATTN_HEADS * 2 * ATTN_HEAD_DIM
ATTN_WIDTH = N_ATTN_HEADS * ATTN_V_DIM
ROPE_THETA = 10000.0
Q_BLOCK = 128
CONV_WIDTH = 512
CONV_SIZE = 3
PEER_HEADS = 8
PEER_N_KEYS = 128
PEER_N_EXPERTS = PEER_N_KEYS * PEER_N_KEYS
PEER_HALF_DIM = 128
PEER_TOPK = 16
TOKEN_CHUNK = 128
NORM_EPS = 1e-6
IN_SIZES = (ATTN_QK_WIDTH, ATTN_QK_WIDTH, ATTN_WIDTH, CONV_WIDTH, CONV_WIDTH, CONV_WIDTH, D_MODEL, D_MODEL)
IN_WIDTH = 2 * ATTN_QK_WIDTH + ATTN_WIDTH + 3 * CONV_WIDTH + 2 * D_MODEL

kernel_name = "hybrid_diffattn_shortconv_peer_block"


def rmsnorm(x, w):
    xf = x.astype(jnp.float32)
    y = xf * lax.rsqrt(jnp.mean(xf * xf, axis=-1, keepdims=True) + NORM_EPS)
    return (y * w.astype(jnp.float32)).astype(x.dtype)


def rope_tables(seq, dim):
    inv_freq = 1.0 / (ROPE_THETA ** (jnp.arange(0, dim, 2, dtype=jnp.float32) / dim))
    ang = jnp.arange(seq, dtype=jnp.float32)[:, None] * inv_freq[None, :]
    ang = jnp.concatenate([ang, ang], axis=-1)
    return jnp.cos(ang), jnp.sin(ang)


def apply_rope(t, cos, sin):
    half = t.shape[-1] // 2
    t1, t2 = t[..., :half], t[..., half:]
    rot = jnp.concatenate([-t2, t1], axis=-1)
    c = cos[None, :, None, None, :].astype(t.dtype)
    s = sin[None, :, None, None, :].astype(t.dtype)
    return t * c + rot * s


def diff_attention(q, k, v, lam):
    b, s, h, _, dh = q.shape
    scale = 1.0 / math.sqrt(dh)
    nb = s // Q_BLOCK
    qb = jnp.moveaxis(q.reshape(b, nb, Q_BLOCK, h, 2, dh), 1, 0)

    def block(qblk):
        sc = jnp.einsum('bqhmd,bkhmd->bhmqk', qblk, k).astype(jnp.float32) * scale
        p = jax.nn.softmax(sc, axis=-1)
        diff = p[:, :, 0] - lam * p[:, :, 1]
        return jnp.einsum('bhqk,bkhe->bqhe', diff.astype(v.dtype), v)

    out = lax.map(block, qb)
    return jnp.moveaxis(out, 0, 1).reshape(b, s, h, v.shape[-1])


def short_conv(z, conv_w):
    c = z.shape[-1]
    return lax.conv_general_dilated(
        z, conv_w.reshape(CONV_SIZE, 1, c).astype(z.dtype), window_strides=(1,),
        padding=((CONV_SIZE // 2, CONV_SIZE // 2),),
        dimension_numbers=('NWC', 'WIO', 'NWC'), feature_group_count=c)


def mixer_block(xn, w_in, lq1, lk1, lq2, lk2, subln_w, conv_w, w_pa, w_pb, w_o, cos, sin, lambda_init):
    b, s, _ = xn.shape
    proj = xn @ w_in
    offs, acc = [], 0
    for sz in IN_SIZES[:-1]:
        acc += sz
        offs.append(acc)
    q, k, v, bg, cg, xc, ga, gb = jnp.split(proj, offs, axis=-1)

    q = apply_rope(q.reshape(b, s, N_ATTN_HEADS, 2, ATTN_HEAD_DIM), cos, sin)
    k = apply_rope(k.reshape(b, s, N_ATTN_HEADS, 2, ATTN_HEAD_DIM), cos, sin)
    v = v.reshape(b, s, N_ATTN_HEADS, ATTN_V_DIM)
    lam = (jnp.exp(jnp.sum(lq1.astype(jnp.float32) * lk1.astype(jnp.float32)))
           - jnp.exp(jnp.sum(lq2.astype(jnp.float32) * lk2.astype(jnp.float32))) + lambda_init)
    attn = diff_attention(q, k, v, lam)
    attn = rmsnorm(attn, subln_w) * (1.0 - lambda_init)
    y_attn = attn.reshape(b, s, ATTN_WIDTH) @ w_pa

    y_conv = (bg * short_conv(cg * xc, conv_w)) @ w_pb

    merged = jax.nn.sigmoid(ga) * y_attn + jax.nn.sigmoid(gb) * y_conv
    return merged @ w_o


def peer_ffn(hn, w_query, sub_keys, expert_u, expert_v):
    b, s, d = hn.shape
    chunks = hn.reshape(-1, TOKEN_CHUNK, d)

    def chunk(xc):
        c = xc.shape[0]
        q = (xc @ w_query).reshape(c, PEER_HEADS, 2, PEER_HALF_DIM)
        sc = jnp.einsum('chpd,hpnd->chpn', q, sub_keys).astype(jnp.float32)
        s1, i1 = lax.top_k(sc[:, :, 0], PEER_TOPK)
        s2, i2 = lax.top_k(sc[:, :, 1], PEER_TOPK)
        cand = (s1[..., :, None] + s2[..., None, :]).reshape(c, PEER_HEADS, PEER_TOPK * PEER_TOPK)
        cidx = (i1[..., :, None] * PEER_N_KEYS + i2[..., None, :]).reshape(c, PEER_HEADS, PEER_TOPK * PEER_TOPK)
        top, pos = lax.top_k(cand, PEER_TOPK)
        eidx = jnp.take_along_axis(cidx, pos, axis=-1)
        g = jax.nn.softmax(top, axis=-1)
        u = expert_u[eidx]
        a = jax.nn.gelu(jnp.einsum('chkd,cd->chk', u, xc), approximate=False)
        vv = expert_v[eidx]
        return jnp.einsum('chk,chkd->cd', (g.astype(a.dtype) * a), vv)

    out = lax.map(chunk, chunks)
    return out.reshape(b, s, d)


def setup_inputs(seed: int = 0) -> dict:
    key = jax.random.key(seed)
    ks = jax.random.split(key, 20)
    f32 = jnp.float32
    n = lambda k, shape, sc: jax.random.normal(k, shape, f32) * sc
    return {
        "x": n(ks[0], (BATCH, SEQ, D_MODEL), 1.0),
        "attn_norm_w": 1.0 + n(ks[1], (DEPTH, D_MODEL), 0.01),
        "w_in": n(ks[2], (DEPTH, D_MODEL, IN_WIDTH), D_MODEL ** -0.5),
        "lambda_q1": n(ks[3], (DEPTH, ATTN_HEAD_DIM), 0.1),
        "lambda_k1": n(ks[4], (DEPTH, ATTN_HEAD_DIM), 0.1),
        "lambda_q2": n(ks[5], (DEPTH, ATTN_HEAD_DIM), 0.1),
        "lambda_k2": n(ks[6], (DEPTH, ATTN_HEAD_DIM), 0.1),
        "subln_w": 1.0 + n(ks[7], (DEPTH, ATTN_V_DIM), 0.01),
        "conv_w": n(ks[8], (DEPTH, CONV_SIZE, CONV_WIDTH), CONV_SIZE ** -0.5),
        "w_proj_attn": n(ks[9], (DEPTH, ATTN_WIDTH, D_MODEL), ATTN_WIDTH ** -0.5),
        "w_proj_conv": n(ks[10], (DEPTH, CONV_WIDTH, D_MODEL), CONV_WIDTH ** -0.5),
        "w_out": n(ks[11], (DEPTH, D_MODEL, D_MODEL), D_MODEL ** -0.5),
        "ffn_norm_w": 1.0 + n(ks[12], (DEPTH, D_MODEL), 0.01),
        "w_query": n(ks[13], (DEPTH, D_MODEL, PEER_HEADS * 2 * PEER_HALF_DIM), D_MODEL ** -0.5),
        "sub_keys": n(ks[14], (DEPTH, PEER_HEADS, 2, PEER_N_KEYS, PEER_HALF_DIM), PEER_HALF_DIM ** -0.5),
        "expert_u": n(ks[15], (DEPTH, PEER_N_EXPERTS, D_MODEL), D_MODEL ** -0.5),
        "expert_v": n(ks[16], (DEPTH, PEER_N_EXPERTS, D_MODEL), PEER_HEADS ** -0.5),
        "final_norm_w": 1.0 + n(ks[17], (D_MODEL,), 0.01),
    }


def reference(x, attn_norm_w, w_in, lambda_q1, lambda_k1, lambda_q2, lambda_k2, subln_w, conv_w,
              w_proj_attn, w_proj_conv, w_out, ffn_norm_w, w_query, sub_keys, expert_u, expert_v,
              final_norm_w):
    cos, sin = rope_tables(x.shape[1], ATTN_HEAD_DIM)
    h = x
    for l in range(DEPTH):
        lambda_init = 0.8 - 0.6 * math.exp(-0.3 * l)
        xn = rmsnorm(h, attn_norm_w[l])
        h = h + mixer_block(xn, w_in[l], lambda_q1[l], lambda_k1[l], lambda_q2[l], lambda_k2[l],
                            subln_w[l], conv_w[l], w_proj_attn[l], w_proj_conv[l], w_out[l],
                            cos, sin, lambda_init)
        hn = rmsnorm(h, ffn_norm_w[l])
        h = h + peer_ffn(hn, w_query[l], sub_keys[l], expert_u[l], expert_v[l])
    return rmsnorm(h, final_norm_w)
```

```python
import numpy as np
import ml_dtypes
import concourse.bass as bass
import concourse.mybir as mybir
from concourse.bass_utils import run_bass_kernel_spmd

F32 = mybir.dt.float32
BF16 = mybir.dt.bfloat16
I32 = mybir.dt.int32
U32 = mybir.dt.uint32
U8 = mybir.dt.uint8
AF = mybir.ActivationFunctionType
ALU = mybir.AluOpType
AX = mybir.AxisListType
DTSIZE = {F32: 4, BF16: 2, I32: 4, U32: 4, U8: 1}


STRICT = True


class Reg:
    __slots__ = ("name", "last_w", "readers", "disjoint")

    def __init__(self, name="", disjoint=False):
        self.name = name
        self.last_w = None
        self.readers = []
        self.disjoint = disjoint


class Eng:
    def __init__(self, fw, name, h, ndma=0):
        self.fw = fw
        self.name = name
        self.h = h
        self.sem = fw.nc.alloc_semaphore("s_" + name)
        self.count = 0
        self.waited = {}
        self.dsems = [fw.nc.alloc_semaphore("d_%s%d" % (name, i)) for i in range(ndma)]
        self.dcount = [0] * ndma
        self.drr = 0

    def wait(self, ev):
        if ev is None:
            return
        _, sem, val = ev
        k = id(sem)
        if self.waited.get(k, 0) >= val:
            return
        self.h.wait_ge(sem, val)
        self.waited[k] = val


class FW:
    def __init__(self, nc, arena_bytes=200 * 1024):
        self.nc = nc
        self.pe = Eng(self, "pe", nc.tensor)
        self.act = Eng(self, "act", nc.scalar)
        self.dve = Eng(self, "dve", nc.vector)
        self.pool = Eng(self, "pool", nc.gpsimd, ndma=12)
        self.sp = Eng(self, "sp", nc.sync, ndma=8)
        self.engs = [self.pe, self.act, self.dve, self.pool, self.sp]
        self.arena = nc.alloc_sbuf_tensor("arena", [128, arena_bytes], U8)
        self.arena_bytes = arena_bytes
        self.top = 0
        self.ps_tensors = []

    def mark(self):
        return self.top

    def release(self, m):
        self.top = m

    def alloc(self, shape, dtype, parts=128):
        n = int(np.prod(shape)) * DTSIZE[dtype]
        n_al = (n + 63) // 64 * 64
        off = self.top
        assert off + n_al <= self.arena_bytes, ("SBUF arena overflow", off, n_al)
        self.top = off + n_al
        ap = self.arena[0:parts, off:off + n].bitcast(dtype)
        if len(shape) > 1:
            names = " ".join("d%d" % i for i in range(len(shape)))
            kw = {"d%d" % i: int(s) for i, s in enumerate(shape)}
            ap = ap.rearrange("p (%s) -> p %s" % (names, names), **kw)
        return ap

    def alloc_at(self, off, shape, dtype, parts=128):
        n = int(np.prod(shape)) * DTSIZE[dtype]
        ap = self.arena[0:parts, off:off + n].bitcast(dtype)
        if len(shape) > 1:
            names = " ".join("d%d" % i for i in range(len(shape)))
            kw = {"d%d" % i: int(s) for i, s in enumerate(shape)}
            ap = ap.rearrange("p (%s) -> p %s" % (names, names), **kw)
        return ap

    def _deps(self, eng, reads, writes, is_dma=False):
        inorder = (not is_dma) and (eng.name == "pe" or (not STRICT and eng.name in ("act", "dve")))
        deps = []
        for r in reads:
            if r.last_w is not None:
                deps.append(r.last_w)
        for w in writes:
            if w.last_w is not None:
                same = (not is_dma) and w.last_w[0] == eng.name
                if not (same and (inorder or w.disjoint)):
                    deps.append(w.last_w)
            for rd in w.readers:
                if inorder and rd[0] == eng.name:
                    continue
                deps.append(rd)
        return deps

    def op(self, eng, fn, reads=(), writes=()):
        deps = self._deps(eng, reads, writes)
        for d in deps:
            if eng.name == "pe" and d[0] == "pe":
                continue
            eng.wait(d)
        ins = fn(eng.h)
        eng.count += 1
        ins.then_inc(eng.sem, 1)
        ev = (eng.name, eng.sem, eng.count)
        self._record(ev, reads, writes)
        return ev

    def _record(self, ev, reads, writes):
        for r in reads:
            r.readers = [x for x in r.readers if x[1] is not ev[1]] + [ev]
        for w in writes:
            w.last_w = ev
            w.readers = []

    def dma(self, eng, fn, reads=(), writes=()):
        deps = self._deps(eng, reads, writes, is_dma=True)
        for d in deps:
            eng.wait(d)
        i = eng.drr
        eng.drr = (i + 1) % len(eng.dsems)
        sem = eng.dsems[i]
        if eng.dcount[i] > 0:
            eng.wait(("dma", sem, eng.dcount[i]))
        ins = fn(eng.h)
        eng.dcount[i] += 16
        ins.then_inc(sem, 16)
        ev = ("dma", sem, eng.dcount[i])
        self._record(ev, reads, writes)
        return ev

    def barrier(self):
        evs = []
        for e in self.engs:
            if e.count:
                evs.append((e.name, e.sem, e.count))
            for i, s in enumerate(e.dsems):
                if e.dcount[i]:
                    evs.append(("dma", s, e.dcount[i]))
        for e in self.engs:
            for ev in evs:
                e.wait(ev)

    def finish(self, out_evs):
        for ev in out_evs:
            self.sp.wait(ev)
            self.pool.wait(ev)

D_MODEL = 1024
SEQ = 4096
NB = 4
T_OWN = 2048
EPS = 1e-6
LAMBDA_INIT = 0.8 - 0.6 * 1.0


def _regs(n, name):
    return [Reg("%s%d" % (name, i)) for i in range(n)]


RSTEPS_CFG = [3]
P4STOP = [0]


def build_program(dbg=False):
    nc = bass.Bass("TRN2", target_bir_lowering=False)

    def DI(name, shape, dt=F32):
        return nc.dram_tensor(name, list(shape), dt, kind="ExternalInput").ap()

    xs_d = DI("xs", [4096, 1024])
    cos_d = DI("cosT", [128, 4096])
    sin_d = DI("sinT", [128, 4096])
    flags_d = DI("flags", [128, 2])
    ident_d = DI("ident", [128, 128])
    iota_d = DI("iota256", [128, 256])
    anw_d = DI("anw", [128, 8])
    fnw_d = DI("fnw", [128, 8])
    fnw_rep_d = DI("fnw_rep", [128, 1024])
    finw_rep_d = DI("finw_rep", [128, 1024])
    subln_d = DI("subln_rep", [128, 128])
    subcol_d = DI("subcol", [128, 1])
    lam_d = DI("lam_in", [128, 4, 64])
    convw_d = DI("convw", [128, 4, 3])
    wA_d = DI("wA", [4, 8, 128, 640])
    wC_d = DI("wC", [4, 8, 128, 384])
    wG_d = DI("wG", [8, 8, 128, 256])
    wpa_d = DI("wpa", [4, 128, 1024])
    wpb_d = DI("wpb", [4, 128, 1024])
    wo_d = DI("wo", [8, 128, 1024])
    wq_d = DI("wq", [8, 128, 2048])
    sk_d = DI("skT", [128, 16, 128])
    eu_d = DI("expert_u", [16384, 1024])
    ev_d = DI("expert_v", [16384, 1024])
    uvs_d = nc.dram_tensor("uvs", [16384, 2048], BF16, kind="Internal").ap()
    out_d = nc.dram_tensor("out", [T_OWN, 1024], F32, kind="ExternalOutput").ap()
    dbg_d = {}
    if dbg:
        dbg_d["attnT"] = nc.dram_tensor("dbg_attnT", [128, 4, 2048], F32, kind="ExternalOutput").ap()
        dbg_d["h"] = nc.dram_tensor("dbg_h", [128, 16, 1024], F32, kind="ExternalOutput").ap()
        dbg_d["eidx"] = nc.dram_tensor("dbg_eidx", [128, 128], I32, kind="ExternalOutput").ap()
        dbg_d["g"] = nc.dram_tensor("dbg_g", [128, 128], F32, kind="ExternalOutput").ap()
        dbg_d["xnT"] = nc.dram_tensor("dbg_xnT", [128, 8, 2048], F32, kind="ExternalOutput").ap()
        dbg_d["uvs"] = nc.dram_tensor("dbg_uvs", [3, 128, 2048], BF16, kind="ExternalOutput").ap()

    fw = FW(nc, arena_bytes=207 * 1024)
    sp, pool, act, dve, pe = fw.sp, fw.pool, fw.act, fw.dve, fw.pe
    ps = [nc.alloc_psum_tensor("ps%d" % i, [128, 512], F32) for i in range(6)]
    pt = [nc.alloc_psum_tensor("pt%d" % i, [128, 1024], BF16) for i in range(2)]
    PS = _regs(6, "ps")
    PT = _regs(2, "pt")
    outs = []

    def dma_in(dst, src, reg, eng=None):
        return fw.dma(eng or sp, lambda e: e.dma_start(out=dst, in_=src), writes=[reg])

    def dbg_dump(name, src_ap, reg, shape):
        if not dbg:
            return
        dst = dbg_d[name]
        outs.append(fw.dma(sp, lambda e: e.dma_start(out=dst, in_=src_ap), reads=[reg]))

    ident_f = fw.alloc([128], F32); ident = fw.alloc([128], BF16)
    anw = fw.alloc([8], F32); fnw = fw.alloc([8], F32)
    subln = fw.alloc([128], F32); flags = fw.alloc([2], F32)
    lam_in = fw.alloc([4, 64], F32); lam_t = fw.alloc([8], F32); lam_j = fw.alloc([64], F32)
    convw = fw.alloc([4, 3], F32)
    RC = Reg("consts")
    for dst, src in ((ident_f, ident_d), (anw, anw_d), (fnw, fnw_d), (subln, subln_d), (flags, flags_d),
                     (lam_in, lam_d), (convw, convw_d)):
        dma_in(dst, src, RC)
    RCI = Reg("ident")
    fw.barrier()
    fw.op(dve, lambda e: e.tensor_copy(out=ident, in_=ident_f), reads=[RC], writes=[RCI])
    fw.op(dve, lambda e: e.tensor_scalar(out=subln, in0=subln, scalar1=1.0 - LAMBDA_INIT, scalar2=None, op0=ALU.mult), reads=[RC], writes=[RC])
    fw.op(dve, lambda e: e.scalar_tensor_tensor(out=lam_j, in0=lam_in[:, 0, :], scalar=1.0, in1=lam_in[:, 1, :], op0=ALU.mult, op1=ALU.mult, accum_out=lam_t[:, 0:1]), reads=[RC], writes=[RC])
    fw.op(dve, lambda e: e.scalar_tensor_tensor(out=lam_j, in0=lam_in[:, 2, :], scalar=1.0, in1=lam_in[:, 3, :], op0=ALU.mult, op1=ALU.mult, accum_out=lam_t[:, 1:2]), reads=[RC], writes=[RC])
    fw.op(act, lambda e: e.activation(out=lam_t[:, 2:4], in_=lam_t[:, 0:2], func=AF.Exp), reads=[RC], writes=[RC])
    fw.op(dve, lambda e: e.tensor_tensor(out=lam_t[:, 4:5], in0=lam_t[:, 3:4], in1=lam_t[:, 2:3], op=ALU.subtract), reads=[RC], writes=[RC])
    fw.op(dve, lambda e: e.tensor_scalar(out=lam_t[:, 4:5], in0=lam_t[:, 4:5], scalar1=-LAMBDA_INIT, scalar2=None, op0=ALU.add), reads=[RC], writes=[RC])
    neglam = lam_t[:, 4:5]

    off_own = fw.top
    xnT_own = fw.alloc([8, 2048], BF16)
    attnT = fw.alloc([4, 2048], BF16)
    ucT = fw.alloc([4, 2048], BF16)
    off_oth = fw.top
    xnT_oth = fw.alloc([8, 2048], BF16)
    m_pers = fw.mark()
    RXN = [Reg("xnT%d" % i, disjoint=True) for i in range(32)]
    stat = [fw.alloc([4], F32) for _ in range(4)]
    RST = _regs(4, "stat")
    m_tmp = fw.mark()

    def rms_rstd(src_ap, n, st, rst, junk, rjunk, src_regs):
        fw.op(act, lambda e: e.activation(out=junk, in_=src_ap, func=AF.Square, accum_out=st[:, 0:1]), reads=src_regs, writes=[rjunk, rst])
        fw.op(dve, lambda e: e.tensor_scalar(out=st[:, 1:2], in0=st[:, 0:1], scalar1=1.0 / n, scalar2=EPS, op0=ALU.mult, op1=ALU.add), reads=[rst], writes=[rst])
        fw.op(act, lambda e: e.activation(out=st[:, 3:4], in_=st[:, 1:2], func=AF.Ln), reads=[rst], writes=[rst])
        fw.op(act, lambda e: e.activation(out=st[:, 2:3], in_=st[:, 3:4], func=AF.Exp, scale=-0.5), reads=[rst], writes=[rst])

    def norm_transpose(load_fn, nblk, dst_fn, wcol, regs_dst, post_fn=None):
        xbuf = [fw.alloc([1024], F32) for _ in range(4)]; RX = _regs(4, "xbuf")
        xnb = [fw.alloc([1024], BF16) for _ in range(2)]; RXB = _regs(2, "xnb")
        junk = fw.alloc([1024], BF16); RJ = Reg("junk")
        srcs = {}

        def stage_a(blk):
            xs = xbuf[blk % 4]; rx = RX[blk % 4]
            src_ap, src_regs = load_fn(blk, xs, rx)
            srcs[blk] = (src_ap, src_regs)
            st = stat[blk % 4]; rst = RST[blk % 4]
            fw.op(act, lambda e: e.activation(out=junk, in_=src_ap, func=AF.Square, accum_out=st[:, 0:1]), reads=src_regs, writes=[RJ, rst])
            fw.op(dve, lambda e: e.tensor_scalar(out=st[:, 1:2], in0=st[:, 0:1], scalar1=1.0 / 1024, scalar2=EPS, op0=ALU.mult, op1=ALU.add), reads=[rst], writes=[rst])

        def stage_b(blk):
            src_ap, src_regs = srcs.pop(blk)
            st = stat[blk % 4]; rst = RST[blk % 4]
            fw.op(act, lambda e: e.activation(out=st[:, 3:4], in_=st[:, 1:2], func=AF.Ln), reads=[rst], writes=[rst])
            fw.op(act, lambda e: e.activation(out=st[:, 2:3], in_=st[:, 3:4], func=AF.Exp, scale=-0.5), reads=[rst], writes=[rst])
            if post_fn is not None:
                post_fn(blk, st, rst)
            xn = xnb[blk % 2]; rxn = RXB[blk % 2]
            fw.op(act, lambda e: e.activation(out=xn, in_=src_ap, func=AF.Copy, scale=st[:, 2:3]), reads=src_regs + [rst], writes=[rxn])
            p = pt[blk % 2]; rp = PT[blk % 2]
            for kc in range(8):
                fw.op(pe, lambda e: e.transpose(out=p[:, kc * 128:(kc + 1) * 128], in_=xn[:, kc * 128:(kc + 1) * 128], identity=ident), reads=[rxn, RCI], writes=[rp])
            for kc in range(8):
                fw.op(dve, lambda e: e.tensor_scalar(out=dst_fn(blk, kc), in0=p[:, kc * 128:(kc + 1) * 128], scalar1=wcol[:, kc:kc + 1], scalar2=None, op0=ALU.mult), reads=[rp, RC], writes=[regs_dst[blk]])

        stage_a(0)
        for blk in range(nblk):
            if blk + 1 < nblk:
                stage_a(blk + 1)
            stage_b(blk)

    def load_x(blk, xs, rx):
        dma_in(xs, xs_d[blk * 128:(blk + 1) * 128, :], rx)
        return xs, [rx]

    def xn_dst(blk, kc):
        if blk < 16:
            return xnT_own[:, kc, blk * 128:(blk + 1) * 128]
        return xnT_oth[:, kc, (blk - 16) * 128:(blk - 15) * 128]

    norm_transpose(load_x, 32, xn_dst, anw, RXN)
    fw.release(m_tmp)
    fw.barrier()

    def xt_tile(t, kc):
        if t < 4:
            return xnT_own[:, kc, t * 512:(t + 1) * 512]
        return xnT_oth[:, kc, (t - 4) * 512:(t - 3) * 512]

    def xt_regs(t):
        return RXN[t * 4:(t + 1) * 4]

    cosT = fw.alloc([4096], F32); sinT = fw.alloc([4096], F32); RTAB = Reg("tab")
    dma_in(cosT, cos_d, RTAB); dma_in(sinT, sin_d, RTAB)
    subcol = fw.alloc([1], F32)
    dma_in(subcol, subcol_d, RC)
    ones_bf = fw.alloc([128], BF16); RONE = Reg("ones")
    fw.barrier()
    fw.op(dve, lambda e: e.tensor_scalar(out=subcol, in0=subcol, scalar1=1.0 - LAMBDA_INIT, scalar2=None, op0=ALU.mult), reads=[RC], writes=[RC])
    fw.op(pool, lambda e: e.memset(ones_bf, 1.0), writes=[RONE])
    wa = fw.alloc([8, 640], BF16); RWA = _regs(8, "wa")
    kT = fw.alloc([4096], BF16); RK = Reg("kT")
    qT = fw.alloc([2048], BF16); RQ = Reg("qT")
    vx = fw.alloc([4096], BF16); RV = Reg("vx")
    t1b = [fw.alloc([512], F32) for _ in range(2)]; RT1 = _regs(2, "t1")
    t2b = [fw.alloc([512], F32) for _ in range(2)]; RT2 = _regs(2, "t2")
    pTb = [fw.alloc([1024], BF16) for _ in range(3)]; RPT = _regs(3, "pT")
    accp = fw.alloc([1024], F32); RACP = Reg("accp")
    rz = fw.alloc([1024], F32); RRZ = Reg("rz")
    RAT = Reg("attnT")
    ropei = [0]
    cvt = fw.alloc([2, 2048], BF16)
    eu_v = eu_d.rearrange("(c p j) d -> c p j d", p=128, j=2)
    ev_v = ev_d.rearrange("(c p j) d -> c p j d", p=128, j=2)
    uvs_v = uvs_d.rearrange("(c p j) d -> c p j d", p=128, j=2)
    RCVu = Reg("cvtu"); RCVv = Reg("cvtv")

    def convert_chunk(c):
        fw.dma(pool, lambda e: e.dma_start(out=cvt[:, :, 0:1024], in_=eu_v[c]), writes=[RCVu])
        fw.dma(pool, lambda e: e.dma_start(out=cvt[:, :, 1024:2048], in_=ev_v[c]), writes=[RCVv])
        fw.dma(sp, lambda e: e.dma_start(out=uvs_v[c], in_=cvt), reads=[RCVu, RCVv])

    def rope(bank_a, ra, bank_b, rb, t, dst, rdst):
        i = ropei[0] % 2; ropei[0] += 1
        fw.op(dve, lambda e: e.tensor_tensor(out=t1b[i], in0=bank_a[:, 0:512], in1=cosT[:, t * 512:(t + 1) * 512], op=ALU.mult), reads=[ra, RTAB], writes=[RT1[i]])
        fw.op(dve, lambda e: e.tensor_tensor(out=t2b[i], in0=bank_b[:, 0:512], in1=sinT[:, t * 512:(t + 1) * 512], op=ALU.mult), reads=[rb, RTAB], writes=[RT2[i]])
        fw.op(pool, lambda e: e.tensor_tensor(out=dst, in0=t1b[i], in1=t2b[i], op=ALU.add), reads=[RT1[i], RT2[i]], writes=[rdst])

    cvi = [0]
    for h in range(4):
        for kc in range(8):
            fw.dma(pool, lambda e: e.dma_start(out=wa[:, kc, :], in_=wA_d[h, kc]), writes=[RWA[kc]])
        for t in range(8):
            groups = [(256, 0), (384, 1)] + ([(0, 2), (128, 3)] if t < 4 else [])
            for col0, bnk in groups:
                for kc in range(8):
                    fw.op(pe, lambda e: e.matmul(out=ps[bnk][:, 0:512], lhsT=wa[:, kc, col0:col0 + 128], rhs=xt_tile(t, kc), start=(kc == 0), stop=(kc == 7)), reads=[RWA[kc]] + xt_regs(t), writes=[PS[bnk]])
            rope(ps[0], PS[0], ps[1], PS[1], t, kT[:, t * 512:(t + 1) * 512], RK)
            if t < 4:
                rope(ps[2], PS[2], ps[3], PS[3], t, qT[:, t * 512:(t + 1) * 512], RQ)
            for j in range(4):
                for kc in range(8):
                    fw.op(pe, lambda e: e.matmul(out=ps[4][:, j * 128:(j + 1) * 128], lhsT=xt_tile(t, kc)[:, j * 128:(j + 1) * 128], rhs=wa[:, kc, 512:640], start=(kc == 0), stop=(kc == 7)), reads=[RWA[kc]] + xt_regs(t), writes=[PS[4]])
            fw.op(act, lambda e: e.activation(out=vx[:, t * 512:(t + 1) * 512], in_=ps[4][:, 0:512], func=AF.Copy), reads=[PS[4]], writes=[RV])
        for qt in range(4):
            q0 = qt * 512
            for _ in range(4):
                convert_chunk(cvi[0]); cvi[0] += 1

            def QK(kb):
                for m in range(2):
                    bnk = (kb % 2) * 2 + m
                    fw.op(pe, lambda e: e.matmul(out=ps[bnk][:, 0:512], lhsT=kT[m * 64:(m + 1) * 64, kb * 128:(kb + 1) * 128], rhs=qT[m * 64:(m + 1) * 64, q0:q0 + 512], start=True, stop=True), reads=[RK, RQ], writes=[PS[bnk]])

            def EXP(kb):
                for m in range(2):
                    bnk = (kb % 2) * 2 + m
                    fw.op(act, lambda e: e.activation(out=pTb[kb % 3][:, m * 512:(m + 1) * 512], in_=ps[bnk][:, 0:512], func=AF.Exp, scale=0.125), reads=[PS[bnk]], writes=[RPT[kb % 3]])

            def PV(kb):
                for m in range(2):
                    fw.op(pe, lambda e: e.matmul(out=ps[4 + m][:, 0:512], lhsT=vx[:, kb * 128:(kb + 1) * 128], rhs=pTb[kb % 3][:, m * 512:(m + 1) * 512], start=(kb == 0), stop=(kb == 31)), reads=[RPT[kb % 3], RV], writes=[PS[4 + m]])
                if kb == 0:
                    fw.op(dve, lambda e: e.tensor_copy(out=accp, in_=pTb[kb % 3]), reads=[RPT[kb % 3]], writes=[RACP])
                else:
                    fw.op(dve, lambda e: e.tensor_tensor(out=accp, in0=accp, in1=pTb[kb % 3], op=ALU.add), reads=[RPT[kb % 3], RACP], writes=[RACP])

            for kb in range(33):
                if kb < 32:
                    QK(kb); EXP(kb)
                if kb >= 1:
                    PV(kb - 1)
            hi, lo = pTb[0], pTb[1]
            fw.op(dve, lambda e: e.tensor_copy(out=hi, in_=accp), reads=[RACP], writes=[RPT[0]])
            fw.op(dve, lambda e: e.tensor_tensor(out=lo, in0=accp, in1=hi, op=ALU.subtract), reads=[RACP, RPT[0]], writes=[RPT[1]])
            for m in range(2):
                fw.op(pe, lambda e: e.matmul(out=ps[m][:, 0:512], lhsT=ones_bf, rhs=hi[:, m * 512:(m + 1) * 512], start=True, stop=False), reads=[RONE, RPT[0]], writes=[PS[m]])
                fw.op(pe, lambda e: e.matmul(out=ps[m][:, 0:512], lhsT=ones_bf, rhs=lo[:, m * 512:(m + 1) * 512], start=False, stop=True), reads=[RONE, RPT[1]], writes=[PS[m]])
                fw.op(dve, lambda e: e.reciprocal(out=rz[:, m * 512:(m + 1) * 512], in_=ps[m][:, 0:512]), reads=[PS[m]], writes=[RRZ])
            ta, tb2, tc, td = t1b[0], t2b[0], t1b[1], t2b[1]
            rta, rtb, rtc, rtd = RT1[0], RT2[0], RT1[1], RT2[1]
            fw.op(dve, lambda e: e.tensor_tensor(out=ta, in0=ps[4][:, 0:512], in1=rz[:, 0:512], op=ALU.mult), reads=[PS[4], RRZ], writes=[rta])
            fw.op(dve, lambda e: e.tensor_tensor(out=tb2, in0=ps[5][:, 0:512], in1=rz[:, 512:1024], op=ALU.mult), reads=[PS[5], RRZ], writes=[rtb])
            fw.op(dve, lambda e: e.scalar_tensor_tensor(out=tc, in0=tb2, scalar=neglam, in1=ta, op0=ALU.mult, op1=ALU.add), reads=[rtb, rta, RC], writes=[rtc])
            sq = pTb[2][:, 0:512]
            fw.op(act, lambda e: e.activation(out=sq, in_=tc, func=AF.Square), reads=[rtc], writes=[RPT[2]])
            fw.op(pe, lambda e: e.matmul(out=ps[2][:, 0:512], lhsT=ones_bf, rhs=sq, start=True, stop=True), reads=[RONE, RPT[2]], writes=[PS[2]])
            fw.op(dve, lambda e: e.tensor_scalar(out=td, in0=ps[2][:, 0:512], scalar1=1.0 / 128, scalar2=EPS, op0=ALU.mult, op1=ALU.add), reads=[PS[2]], writes=[rtd])
            fw.op(act, lambda e: e.activation(out=td, in_=td, func=AF.Ln), reads=[rtd], writes=[rtd])
            fw.op(act, lambda e: e.activation(out=td, in_=td, func=AF.Exp, scale=-0.5), reads=[rtd], writes=[rtd])
            fw.op(dve, lambda e: e.scalar_tensor_tensor(out=attnT[:, h, q0:q0 + 512], in0=tc, scalar=subcol[:, 0:1], in1=td, op0=ALU.mult, op1=ALU.mult), reads=[rtc, rtd, RC], writes=[RAT])
    fw.barrier()
    if dbg:
        stg = fw.alloc([2048], F32); RSTG = Reg("stg")
        for hh in range(4):
            fw.op(dve, lambda e: e.tensor_copy(out=stg, in_=attnT[:, hh, :]), reads=[RAT], writes=[RSTG])
            outs.append(fw.dma(sp, lambda e: e.dma_start(out=dbg_d["attnT"][:, hh, :], in_=stg), reads=[RSTG]))
        for kc in range(8):
            fw.op(dve, lambda e: e.tensor_copy(out=stg, in_=xnT_own[:, kc, :]), reads=RXN[0:16], writes=[RSTG])
            outs.append(fw.dma(sp, lambda e: e.dma_start(out=dbg_d["xnT"][:, kc, :], in_=stg), reads=[RSTG]))
        fw.barrier()
        stb = fw.alloc([2048], BF16); RSTB = Reg("stb")
        for i, r0 in enumerate((0, 640, 16256)):
            fw.dma(sp, lambda e: e.dma_start(out=stb, in_=uvs_d[r0:r0 + 128, :]), writes=[RSTB])
            outs.append(fw.dma(sp, lambda e: e.dma_start(out=dbg_d["uvs"][i], in_=stb), reads=[RSTB]))
        fw.barrier()
    fw.release(m_tmp)

    wcb = [fw.alloc([8, 384], BF16) for _ in range(2)]; RWCb = [_regs(8, "wc%d" % i) for i in range(2)]

    def load_wc(cc):
        for kc in range(8):
            fw.dma(pool, lambda e: e.dma_start(out=wcb[cc % 2][:, kc, :], in_=wC_d[cc, kc]), writes=[RWCb[cc % 2][kc]])

    zT = fw.alloc([2050], F32); RZ = Reg("zT")
    bgs = fw.alloc([2048], F32); RBG = Reg("bgs")
    tmpc = [fw.alloc([512], F32) for _ in range(2)]; RTC = _regs(2, "tmpc")
    c1 = fw.alloc([2048], F32); RC1 = Reg("c1"); c2 = fw.alloc([2048], F32); RC2 = Reg("c2")
    hs = fw.alloc([16], F32); RHS = Reg("hs")
    RUC = Reg("ucT")
    load_wc(0)
    for cc in range(4):
        wc = wcb[cc % 2]; RWC = RWCb[cc % 2]
        if cc + 1 < 4:
            load_wc(cc + 1)
        for t in range(4):
            for gi, col0 in enumerate((128, 256, 0)):
                for kc in range(8):
                    fw.op(pe, lambda e: e.matmul(out=ps[gi][:, 0:512], lhsT=wc[:, kc, col0:col0 + 128], rhs=xt_tile(t, kc), start=(kc == 0), stop=(kc == 7)), reads=[RWC[kc]] + xt_regs(t), writes=[PS[gi]])
            fw.op(act, lambda e: e.activation(out=tmpc[t % 2], in_=ps[0][:, 0:512], func=AF.Copy), reads=[PS[0]], writes=[RTC[t % 2]])
            fw.op(dve, lambda e: e.tensor_tensor(out=zT[:, 1 + t * 512:1 + (t + 1) * 512], in0=ps[1][:, 0:512], in1=tmpc[t % 2], op=ALU.mult), reads=[PS[1], RTC[t % 2]], writes=[RZ])
            fw.op(act, lambda e: e.activation(out=bgs[:, t * 512:(t + 1) * 512], in_=ps[2][:, 0:512], func=AF.Copy), reads=[PS[2]], writes=[RBG])
        for gi, (col0, tok) in enumerate(((128, 0), (256, 0), (128, 2044), (256, 2044))):
            for kc in range(8):
                fw.op(pe, lambda e: e.matmul(out=ps[3][:, gi * 4:gi * 4 + 4], lhsT=wc[:, kc, col0:col0 + 128], rhs=xnT_oth[:, kc, tok:tok + 4], start=(kc == 0), stop=(kc == 7)), reads=[RWC[kc]] + RXN[16:32], writes=[PS[3]])
        fw.op(dve, lambda e: e.tensor_copy(out=hs, in_=ps[3][:, 0:16]), reads=[PS[3]], writes=[RHS])
        fw.op(dve, lambda e: e.scalar_tensor_tensor(out=zT[:, 2049:2050], in0=hs[:, 0:1], scalar=flags[:, 1:2], in1=hs[:, 4:5], op0=ALU.mult, op1=ALU.mult), reads=[RHS, RC], writes=[RZ])
        fw.op(dve, lambda e: e.scalar_tensor_tensor(out=zT[:, 0:1], in0=hs[:, 11:12], scalar=flags[:, 0:1], in1=hs[:, 15:16], op0=ALU.mult, op1=ALU.mult), reads=[RHS, RC], writes=[RZ])
        fw.op(dve, lambda e: e.tensor_scalar(out=c1, in0=zT[:, 0:2048], scalar1=convw[:, cc, 0:1], scalar2=None, op0=ALU.mult), reads=[RZ, RC], writes=[RC1])
        fw.op(dve, lambda e: e.scalar_tensor_tensor(out=c2, in0=zT[:, 1:2049], scalar=convw[:, cc, 1:2], in1=c1, op0=ALU.mult, op1=ALU.add), reads=[RZ, RC, RC1], writes=[RC2])
        fw.op(dve, lambda e: e.scalar_tensor_tensor(out=c1, in0=zT[:, 2:2050], scalar=convw[:, cc, 2:3], in1=c2, op0=ALU.mult, op1=ALU.add), reads=[RZ, RC, RC2], writes=[RC1])
        fw.op(dve, lambda e: e.tensor_tensor(out=ucT[:, cc, :], in0=c1, in1=bgs, op=ALU.mult), reads=[RC1, RBG], writes=[RUC])
    fw.barrier()
    fw.release(m_tmp)

    mergedT = xnT_oth; RMG = Reg("merged")
    wpa_sb = fw.alloc([4, 1024], BF16); wpb_sb = fw.alloc([4, 1024], BF16); RWP = _regs(8, "wp")
    for c4 in range(4):
        fw.dma(pool, lambda e: e.dma_start(out=wpa_sb[:, c4, :], in_=wpa_d[c4]), writes=[RWP[c4]])
        fw.dma(pool, lambda e: e.dma_start(out=wpb_sb[:, c4, :], in_=wpb_d[c4]), writes=[RWP[4 + c4]])
    wgb = [fw.alloc([8, 256], BF16) for _ in range(2)]; RWGb = [_regs(8, "wg%d" % i) for i in range(2)]

    def load_wg(dc):
        for kc in range(8):
            fw.dma(pool, lambda e: e.dma_start(out=wgb[dc % 2][:, kc, :], in_=wG_d[dc, kc]), writes=[RWGb[dc % 2][kc]])

    sgAb = [fw.alloc([512], F32) for _ in range(2)]; sgBb = [fw.alloc([512], F32) for _ in range(2)]; RSGb = [_regs(2, "sg%d" % i) for i in range(2)]
    m1b = [fw.alloc([512], F32) for _ in range(2)]; m2b = [fw.alloc([512], F32) for _ in range(2)]; RMb = [_regs(2, "m%d" % i) for i in range(2)]
    load_wg(0)
    for dc in range(8):
        wg = wgb[dc % 2]; RWG = RWGb[dc % 2]
        if dc + 1 < 8:
            load_wg(dc + 1)
        for t in range(4):
            ba, bc = (0, 1) if t % 2 == 0 else (4, 5)
            sgA, sgB, RSG = sgAb[t % 2], sgBb[t % 2], RSGb[t % 2]
            m1, m2, RM = m1b[t % 2], m2b[t % 2], RMb[t % 2]
            for c4 in range(4):
                fw.op(pe, lambda e: e.matmul(out=ps[ba][:, 0:512], lhsT=wpa_sb[:, c4, dc * 128:(dc + 1) * 128], rhs=attnT[:, c4, t * 512:(t + 1) * 512], start=(c4 == 0), stop=(c4 == 3)), reads=[RWP[c4], RAT], writes=[PS[ba]])
            for c4 in range(4):
                fw.op(pe, lambda e: e.matmul(out=ps[bc][:, 0:512], lhsT=wpb_sb[:, c4, dc * 128:(dc + 1) * 128], rhs=ucT[:, c4, t * 512:(t + 1) * 512], start=(c4 == 0), stop=(c4 == 3)), reads=[RWP[4 + c4], RUC], writes=[PS[bc]])
            for gi in range(2):
                for kc in range(8):
                    fw.op(pe, lambda e: e.matmul(out=ps[2 + gi][:, 0:512], lhsT=wg[:, kc, gi * 128:(gi + 1) * 128], rhs=xt_tile(t, kc), start=(kc == 0), stop=(kc == 7)), reads=[RWG[kc]] + xt_regs(t), writes=[PS[2 + gi]])
            fw.op(act, lambda e: e.activation(out=sgA, in_=ps[2][:, 0:512], func=AF.Sigmoid), reads=[PS[2]], writes=[RSG[0]])
            fw.op(act, lambda e: e.activation(out=sgB, in_=ps[3][:, 0:512], func=AF.Sigmoid), reads=[PS[3]], writes=[RSG[1]])
            fw.op(dve, lambda e: e.tensor_tensor(out=m1, in0=ps[ba][:, 0:512], in1=sgA, op=ALU.mult), reads=[PS[ba], RSG[0]], writes=[RM[0]])
            fw.op(dve, lambda e: e.tensor_tensor(out=m2, in0=ps[bc][:, 0:512], in1=sgB, op=ALU.mult), reads=[PS[bc], RSG[1]], writes=[RM[1]])
            fw.op(pool, lambda e: e.tensor_tensor(out=mergedT[:, dc, t * 512:(t + 1) * 512], in0=m1, in1=m2, op=ALU.add), reads=[RM[0], RM[1]], writes=[RMG])
    fw.barrier()
    fw.release(m_tmp)

    hT = fw.alloc_at(off_own, [16, 1024], F32); RH = _regs(16, "h")
    wo_sb = fw.alloc([8, 1024], BF16); RWO = _regs(8, "wo")
    for dc in range(8):
        fw.dma(pool, lambda e: e.dma_start(out=wo_sb[:, dc, :], in_=wo_d[dc]), writes=[RWO[dc]])
    xres = [fw.alloc([1024], F32) for _ in range(2)]; RXR = _regs(2, "xres")
    for blk in range(16):
        dma_in(xres[blk % 2], xs_d[blk * 128:(blk + 1) * 128, :], RXR[blk % 2])
        for half in range(2):
            for dc in range(8):
                fw.op(pe, lambda e: e.matmul(out=ps[half][:, 0:512], lhsT=mergedT[:, dc, blk * 128:(blk + 1) * 128], rhs=wo_sb[:, dc, half * 512:(half + 1) * 512], start=(dc == 0), stop=(dc == 7)), reads=[RMG, RWO[dc]], writes=[PS[half]])
            fw.op(dve, lambda e: e.tensor_tensor(out=hT[:, blk, half * 512:(half + 1) * 512], in0=ps[half][:, 0:512], in1=xres[blk % 2][:, half * 512:(half + 1) * 512], op=ALU.add), reads=[PS[half], RXR[blk % 2]], writes=[RH[blk]])
    fw.barrier()
    fw.release(m_tmp)
    if dbg:
        for blk in range(16):
            outs.append(fw.dma(sp, lambda e: e.dma_start(out=dbg_d["h"][:, blk, :], in_=hT[:, blk, :]), reads=[RH[blk]]))

    wq_sb = fw.alloc_at(off_oth, [8, 2048], BF16); RWQs = _regs(16, "wq")
    for kc in range(8):
        for hf in range(2):
            fw.dma(pool, lambda e: e.dma_start(out=wq_sb[:, kc, hf * 1024:(hf + 1) * 1024], in_=wq_d[kc][:, hf * 1024:(hf + 1) * 1024]), writes=[RWQs[kc * 2 + hf]])
    rstd_all = fw.alloc([16], F32); ss_all = fw.alloc([16], F32); RRS = Reg("rstd_all")
    NR = 4
    eidx_r = [fw.alloc([128], I32) for _ in range(NR)]; REA = _regs(NR, "eidx_r")
    g_r = [fw.alloc([128], F32) for _ in range(NR)]; RGA = _regs(NR, "g_r")
    iota256 = fw.alloc([64], F32)
    fnw_rep = fw.alloc([1024], F32); finw_rep = fw.alloc([1024], F32)
    scs = fw.alloc([2048], F32); RSC = Reg("scs")
    sk_f = scs.rearrange("p (a b) -> p a b", a=16, b=128); sk_b = fw.alloc([16, 128], BF16); RSK = Reg("sk")
    dma_in(iota256, iota_d[:, 0:64], RC); dma_in(fnw_rep, fnw_rep_d, RC); dma_in(finw_rep, finw_rep_d, RC)
    dma_in(sk_f, sk_d, RSK)
    fw.barrier()
    fw.op(dve, lambda e: e.tensor_copy(out=sk_b, in_=sk_f), reads=[RSK], writes=[RSK])
    junkn = fw.alloc([1024], BF16); RJN = Reg("junkn")
    for blk in range(16):
        fw.op(act, lambda e: e.activation(out=junkn, in_=hT[:, blk, :], func=AF.Square, accum_out=ss_all[:, blk:blk + 1]), reads=[RH[blk]], writes=[RJN, RRS])
    fw.op(dve, lambda e: e.tensor_scalar(out=ss_all, in0=ss_all, scalar1=1.0 / 1024, scalar2=EPS, op0=ALU.mult, op1=ALU.add), reads=[RRS], writes=[RRS])
    fw.op(act, lambda e: e.activation(out=ss_all, in_=ss_all, func=AF.Ln), reads=[RRS], writes=[RRS])
    fw.op(act, lambda e: e.activation(out=rstd_all, in_=ss_all, func=AF.Exp, scale=-0.5), reads=[RRS], writes=[RRS])
    fw.barrier()

    if P4STOP[0] == 1:
        fw.barrier(); fw.finish(outs); return nc
    RECTS = [(0, 2, 16, 0), (2, 2, 5, 32), (4, 4, 3, 42), (8, 8, 1, 54)]
    NCAND = 62
    qps = fw.alloc([2048], BF16); RQP = Reg("qps")
    top = fw.alloc([256], F32); RTOPs = [Reg("top%d" % i, disjoint=True) for i in range(16)]
    idxu = fw.alloc([256], U32); RIDXs = [Reg("idxu%d" % i, disjoint=True) for i in range(16)]
    workb = [fw.alloc([128], F32) for _ in range(2)]; RWKb = _regs(2, "workb")
    cand_all = fw.alloc([8, NCAND], F32); cidx_all = fw.alloc([8, NCAND], F32)
    RCDA = Reg("cand_all", disjoint=True); RCIA = Reg("cidx_all", disjoint=True)
    HS = [dict(work2=fw.alloc([NCAND], F32), posu=fw.alloc([16], U32), posf=fw.alloc([16], F32),
               RWK2=Reg("work2"), RPOS=Reg("posu", disjoint=True), RPOSF=Reg("posf")) for _ in range(2)]
    RCTs = [Reg("ctop%d" % i, disjoint=True) for i in range(8)]
    idxf = fw.alloc([256], F32); idxf128 = fw.alloc([256], F32); RIF = Reg("idxf")
    ctop = fw.alloc([128], F32); RCT = Reg("ctop", disjoint=True)
    NJ = 4
    junkc = [fw.alloc([NCAND], F32) for _ in range(NJ)]; RJ2 = _regs(NJ, "junkc")
    eidxf = fw.alloc([128], F32); REI = Reg("eidxf", disjoint=True)
    posu = fw.alloc([16], U32); RPOS = Reg("posu", disjoint=True); posf = fw.alloc([16], F32); RPOSF = Reg("posf")
    negmax = fw.alloc([8], F32); gs = fw.alloc([8], F32); rg = fw.alloc([8], F32); RSM = Reg("sm", disjoint=True)
    xnb = [fw.alloc([1024], BF16) for _ in range(1)]; RXB = _regs(1, "xnb")
    hnb = [fw.alloc([8, 128], BF16) for _ in range(2)]; RHB = [_regs(8, "hnb%d" % i) for i in range(2)]
    jc = [0]

    def route_pre(blk):
        xn = xnb[0]; rxn = RXB[0]
        hb = hnb[blk % 2]; rhb = RHB[blk % 2]
        fw.op(act, lambda e: e.activation(out=xn, in_=hT[:, blk, :], func=AF.Copy, scale=rstd_all[:, blk:blk + 1]), reads=[RH[blk], RRS], writes=[rxn])
        p = pt[blk % 2]; rp = PT[blk % 2]
        for kc in range(8):
            fw.op(pe, lambda e: e.transpose(out=p[:, kc * 128:(kc + 1) * 128], in_=xn[:, kc * 128:(kc + 1) * 128], identity=ident), reads=[rxn, RCI], writes=[rp])
        for kc in range(8):
            fw.op(act, lambda e: e.activation(out=hb[:, kc, :], in_=p[:, kc * 128:(kc + 1) * 128], func=AF.Copy, scale=fnw[:, kc:kc + 1]), reads=[rp, RC], writes=[rhb[kc]])

    def route(blk):
        slot = blk % NR
        gt = g_r[slot]; RG = RGA[slot]
        hb = hnb[blk % 2]; rhb = RHB[blk % 2]
        for g in range(4):
            bank = ps[4 + g % 2]; rb = PS[4 + g % 2]
            for j in range(4):
                hp = g * 4 + j
                for kc in range(8):
                    fw.op(pe, lambda e: e.matmul(out=bank[:, j * 128:(j + 1) * 128], lhsT=wq_sb[:, kc, hp * 128:(hp + 1) * 128], rhs=hb[:, kc, :], start=(kc == 0), stop=(kc == 7)), reads=[RWQs[kc * 2 + hp // 8], rhb[kc]], writes=[rb])
            fw.op(act, lambda e: e.activation(out=qps[:, g * 512:(g + 1) * 512], in_=bank[:, 0:512], func=AF.Copy), reads=[rb], writes=[RQP])
        yield
        for g in range(4):
            bank = ps[4 + g % 2]; rb = PS[4 + g % 2]
            for j in range(4):
                hp = g * 4 + j
                fw.op(pe, lambda e: e.matmul(out=bank[:, j * 128:(j + 1) * 128], lhsT=qps[:, hp * 128:(hp + 1) * 128], rhs=sk_b[:, hp, :], start=True, stop=True), reads=[RQP, RSK], writes=[rb])
            fw.op(act, lambda e: e.activation(out=scs[:, g * 512:(g + 1) * 512], in_=bank[:, 0:512], func=AF.Copy), reads=[rb], writes=[RSC])
        yield
        def topk_chain(hp):
            sv = scs[:, hp * 128:(hp + 1) * 128]
            wk = workb[hp % 2]; rwk = RWKb[hp % 2]
            t8a = top[:, hp * 16:hp * 16 + 8]; t8b = top[:, hp * 16 + 8:hp * 16 + 16]
            i8a = idxu[:, hp * 16:hp * 16 + 8]; i8b = idxu[:, hp * 16 + 8:hp * 16 + 16]
            return [
                lambda: fw.op(dve, lambda e: e.max(out=t8a, in_=sv), reads=[RSC], writes=[RTOPs[hp]]),
                lambda: fw.op(dve, lambda e: e.max_index(out=i8a, in_max=t8a, in_values=sv), reads=[RSC, RTOPs[hp]], writes=[RIDXs[hp]]),
                lambda: fw.op(dve, lambda e: e.match_replace(out=wk, in_to_replace=t8a, in_values=sv, imm_value=-1e30), reads=[RSC, RTOPs[hp]], writes=[rwk]),
                lambda: fw.op(dve, lambda e: e.max(out=t8b, in_=wk), reads=[rwk], writes=[RTOPs[hp]]),
                lambda: fw.op(dve, lambda e: e.max_index(out=i8b, in_max=t8b, in_values=wk), reads=[rwk, RTOPs[hp]], writes=[RIDXs[hp]]),
            ]

        for hp in range(0, 16, 2):
            ca, cb = topk_chain(hp), topk_chain(hp + 1)
            for ta, tb in zip(ca, cb):
                ta(); yield
                tb(); yield
        fw.op(dve, lambda e: e.tensor_copy(out=idxf, in_=idxu), reads=RIDXs, writes=[RIF])
        yield
        fw.op(dve, lambda e: e.tensor_scalar(out=idxf128, in0=idxf, scalar1=128.0, scalar2=None, op0=ALU.mult), reads=[RIF], writes=[RIF])
        yield

        top4 = top.rearrange("p (h t k) -> p h t k", h=8, t=2, k=16)
        if4 = idxf.rearrange("p (h t k) -> p h t k", h=8, t=2, k=16)
        if4s = idxf128.rearrange("p (h t k) -> p h t k", h=8, t=2, k=16)
        for (aa0, na, nb, off) in RECTS:
            cv = cand_all[:, :, off:off + na * nb].rearrange("p h (a b) -> p h a b", a=na, b=nb)
            iv = cidx_all[:, :, off:off + na * nb].rearrange("p h (a b) -> p h a b", a=na, b=nb)
            fw.op(dve, lambda e: e.tensor_tensor(out=cv, in0=top4[:, :, 0, aa0:aa0 + na].unsqueeze(3).to_broadcast([128, 8, na, nb]), in1=top4[:, :, 1, 0:nb].unsqueeze(2).to_broadcast([128, 8, na, nb]), op=ALU.add), reads=RTOPs, writes=[RCDA])
            yield
            fw.op(dve, lambda e: e.tensor_tensor(out=iv, in0=if4s[:, :, 0, aa0:aa0 + na].unsqueeze(3).to_broadcast([128, 8, na, nb]), in1=if4[:, :, 1, 0:nb].unsqueeze(2).to_broadcast([128, 8, na, nb]), op=ALU.add), reads=[RIF], writes=[RCIA])
            yield

        def head_chain(hh):
            S = HS[hh % 2]
            work_h, posu_h, posf_h = S["work2"], S["posu"], S["posf"]
            rwk2, rpos, rposf = S["RWK2"], S["RPOS"], S["RPOSF"]
            cand_h = cand_all[:, hh, :]; cidx_h = cidx_all[:, hh, :]
            rcd, rci = RCDA, RCIA
            c8a = ctop[:, hh * 16:hh * 16 + 8]; c8b = ctop[:, hh * 16 + 8:hh * 16 + 16]
            ops = []
            ops += [
                lambda: fw.op(dve, lambda e: e.max(out=c8a, in_=cand_h), reads=[rcd], writes=[RCTs[hh]]),
                lambda: fw.op(dve, lambda e: e.match_replace(out=work_h, in_to_replace=c8a, in_values=cand_h, imm_value=-1e30), reads=[rcd, RCTs[hh]], writes=[rwk2]),
                lambda: fw.op(dve, lambda e: e.max(out=c8b, in_=work_h), reads=[rwk2], writes=[RCTs[hh]]),
                lambda: fw.op(dve, lambda e: e.max_index(out=posu_h[:, 0:8], in_max=c8a, in_values=cand_h), reads=[rcd, RCTs[hh]], writes=[rpos]),
                lambda: fw.op(dve, lambda e: e.max_index(out=posu_h[:, 8:16], in_max=c8b, in_values=work_h), reads=[rwk2, RCTs[hh]], writes=[rpos]),
                lambda: fw.op(dve, lambda e: e.tensor_copy(out=posf_h, in_=posu_h), reads=[rpos], writes=[rposf]),
            ]
            for k in range(16):
                def mk2(k=k):
                    def f():
                        jj = jc[0] % NJ; jc[0] += 1
                        fw.op(dve, lambda e: e.scalar_tensor_tensor(out=junkc[jj], in0=iota256[:, 0:NCAND], scalar=posf_h[:, k:k + 1], in1=cidx_h, op0=ALU.is_equal, op1=ALU.mult, accum_out=eidxf[:, hh * 16 + k:hh * 16 + k + 1]), reads=[RC, rposf, rci], writes=[RJ2[jj], REI])
                    return f
                ops.append(mk2())
            return ops

        for hh in range(0, 8, 2):
            ca, cb = head_chain(hh), head_chain(hh + 1)
            for ta, tb in zip(ca, cb):
                ta(); yield
                tb(); yield
        fw.op(dve, lambda e: e.tensor_scalar(out=eidxf, in0=eidxf, scalar1=16383.0, scalar2=0.0, op0=ALU.min, op1=ALU.max), reads=[REI], writes=[REI])
        yield
        fw.op(dve, lambda e: e.tensor_copy(out=eidx_r[slot], in_=eidxf), reads=[REI], writes=[REA[slot]])
        yield
        for hh in range(8):
            fw.op(dve, lambda e: e.tensor_scalar(out=negmax[:, hh:hh + 1], in0=ctop[:, hh * 16:hh * 16 + 1], scalar1=-1.0, scalar2=None, op0=ALU.mult), reads=[RCTs[hh]], writes=[RSM])
            yield
        for hh in range(8):
            fw.op(act, lambda e: e.activation(out=gt[:, hh * 16:(hh + 1) * 16], in_=ctop[:, hh * 16:(hh + 1) * 16], func=AF.Exp, bias=negmax[:, hh:hh + 1], accum_out=gs[:, hh:hh + 1]), reads=[RCTs[hh], RSM], writes=[RG, RSM])
        fw.op(dve, lambda e: e.reciprocal(out=rg, in_=gs), reads=[RSM], writes=[RSM])
        yield
        for hh in range(8):
            fw.op(dve, lambda e: e.tensor_scalar(out=gt[:, hh * 16:(hh + 1) * 16], in0=gt[:, hh * 16:(hh + 1) * 16], scalar1=rg[:, hh:hh + 1], scalar2=None, op0=ALU.mult), reads=[RG, RSM], writes=[RG])
            yield

    NG = 10
    uvb = [fw.alloc([2048], BF16) for _ in range(NG)]; RUV = _regs(NG, "uvb")
    ND = 4
    diag = [fw.alloc([128], BF16) for _ in range(ND)]; RDG = _regs(ND, "diag")
    acol = [fw.alloc([4], F32) for _ in range(ND)]; RAC = _regs(ND, "acol")
    hn_tok = [fw.alloc([1024], BF16) for _ in range(1)]; RHT = _regs(1, "hn_tok")
    NJB = 2
    junkb = [fw.alloc([1024], BF16) for _ in range(NJB)]; RJ1 = _regs(NJB, "junkb")
    acc = fw.alloc([1024], F32); RACC = Reg("acc")
    obuf = [fw.alloc([1024], F32) for _ in range(2)]; ROB = _regs(2, "obuf")
    st4 = fw.alloc([4], F32); RS4 = Reg("st4")
    gi = [0]

    def gather(blk):
        tk = slice(blk * 128, (blk + 1) * 128)
        slot = blk % NR
        ht = hn_tok[0]; rht = RHT[0]
        fw.op(dve, lambda e: e.scalar_tensor_tensor(out=ht, in0=hT[:, blk, :], scalar=rstd_all[:, blk:blk + 1], in1=fnw_rep, op0=ALU.mult, op1=ALU.mult), reads=[RH[blk], RRS, RC], writes=[rht])
        pa = (ps[0], ps[1]) if blk % 2 == 0 else (ps[2], ps[3])
        rpa = (PS[0], PS[1]) if blk % 2 == 0 else (PS[2], PS[3])
        for hk in range(128):
            b = gi[0] % NG; d = gi[0] % ND; jb = gi[0] % NJB; gi[0] += 1
            fw.dma(pool, lambda e: e.indirect_dma_start(out=uvb[b], out_offset=None, in_=uvs_d, in_offset=bass.IndirectOffsetOnAxis(ap=eidx_r[slot][:, hk:hk + 1], axis=0)), reads=[REA[slot]], writes=[RUV[b]])
            fw.op(dve, lambda e: e.scalar_tensor_tensor(out=junkb[jb], in0=uvb[b][:, 0:1024], scalar=1.0, in1=ht, op0=ALU.mult, op1=ALU.mult, accum_out=acol[d][:, 0:1]), reads=[RUV[b], rht], writes=[RJ1[jb], RAC[d]])
            fw.op(act, lambda e: e.activation(out=acol[d][:, 1:2], in_=acol[d][:, 0:1], func=AF.Gelu), reads=[RAC[d]], writes=[RAC[d]])
            fw.op(act, lambda e: e.activation(out=acol[d][:, 2:3], in_=acol[d][:, 1:2], func=AF.Copy, scale=g_r[slot][:, hk:hk + 1]), reads=[RAC[d], RGA[slot]], writes=[RAC[d]])
            fw.op(act, lambda e: e.activation(out=diag[d], in_=ident, func=AF.Copy, scale=acol[d][:, 2:3]), reads=[RCI, RAC[d]], writes=[RDG[d]])
            for half in range(2):
                fw.op(pe, lambda e: e.matmul(out=pa[half][:, 0:512], lhsT=diag[d], rhs=uvb[b][:, 1024 + half * 512:1024 + (half + 1) * 512], start=(hk == 0), stop=(hk == 127)), reads=[RDG[d], RUV[b]], writes=[rpa[half]])
            yield
        for half in range(2):
            fw.op(dve, lambda e: e.tensor_tensor(out=acc[:, half * 512:(half + 1) * 512], in0=pa[half][:, 0:512], in1=hT[:, blk, half * 512:(half + 1) * 512], op=ALU.add), reads=[rpa[half], RH[blk]], writes=[RACC])
        ob = obuf[blk % 2]; rob = ROB[blk % 2]
        rms_rstd(acc, 1024, st4, RS4, ob, rob, [RACC])
        fw.op(dve, lambda e: e.scalar_tensor_tensor(out=ob, in0=acc, scalar=st4[:, 2:3], in1=finw_rep, op0=ALU.mult, op1=ALU.mult), reads=[RACC, RS4, RC], writes=[rob])
        outs.append(fw.dma(sp, lambda e: e.dma_start(out=out_d[tk, :], in_=ob), reads=[rob]))

    RSTEPS = RSTEPS_CFG[0]
    if P4STOP[0] == 5:
        def load_h(blk, xs, rx):
            return hT[:, blk, :], [RH[blk]]
        RTMP = _regs(1, "tmpdst")
        norm_transpose(load_h, 1, lambda blk, kc: hnb[0][:, kc, :], fnw, RTMP)
        fw.barrier(); fw.finish(outs); return nc
    if P4STOP[0] == 6:
        xn = xnb[0]
        fw.op(act, lambda e: e.activation(out=xn, in_=hT[:, 0, :], func=AF.Copy, scale=rstd_all[:, 0:1]), reads=[RH[0], RRS], writes=[RXB[0]])
        fw.barrier(); fw.finish(outs); return nc
    if P4STOP[0] in (7, 8, 9):
        xn = xnb[0]
        fw.op(act, lambda e: e.activation(out=xn, in_=hT[:, 0, :], func=AF.Copy, scale=rstd_all[:, 0:1]), reads=[RH[0], RRS], writes=[RXB[0]])
        for kc in range(8):
            fw.op(pe, lambda e: e.transpose(out=pt[0][:, kc * 128:(kc + 1) * 128], in_=xn[:, kc * 128:(kc + 1) * 128], identity=ident), reads=[RXB[0], RCI], writes=[PT[0]])
        if P4STOP[0] == 8:
            for kc in range(0, 8, 2):
                fw.op(dve, lambda e: e.tensor_scalar(out=hnb[0][:, kc, :], in0=pt[0][:, kc * 128:(kc + 1) * 128], scalar1=fnw[:, kc:kc + 1], scalar2=None, op0=ALU.mult), reads=[PT[0], RC], writes=[RHB[0][kc]])
        if P4STOP[0] == 9:
            for kc in range(1, 8, 2):
                fw.op(act, lambda e: e.activation(out=hnb[0][:, kc, :], in_=pt[0][:, kc * 128:(kc + 1) * 128], func=AF.Copy, scale=fnw[:, kc:kc + 1]), reads=[PT[0], RC], writes=[RHB[0][kc]])
        fw.barrier(); fw.finish(outs); return nc
    route_pre(0)
    if P4STOP[0] == 2:
        fw.barrier(); fw.finish(outs); return nc
    for _ in route(0):
        pass
    if P4STOP[0] == 3:
        fw.barrier(); fw.finish(outs); return nc
    if P4STOP[0] == 4:
        for _ in gather(0):
            pass
        fw.barrier(); fw.finish(outs); return nc
    for blk in range(16):
        if blk + 1 < 16:
            route_pre(blk + 1)
        rr = route(blk + 1) if blk + 1 < 16 else None
        for _ in gather(blk):
            if rr is not None:
                for _i in range(RSTEPS):
                    try:
                        next(rr)
                    except StopIteration:
                        rr = None
                        break
        if rr is not None:
            for _ in rr:
                pass
    fw.finish(outs)
    return nc


_CACHE = {}


def _prep_shared(inp):
    f = np.float32
    w_in = np.asarray(inp["w_in"], f)[0]
    perm = np.array([m * 64 + (d + 32) % 64 for m in range(2) for d in range(64)])
    wA = []
    for h in range(4):
        q = w_in[:, h * 128:(h + 1) * 128]
        k = w_in[:, 512 + h * 128:512 + (h + 1) * 128]
        v = w_in[:, 1024 + h * 128:1024 + (h + 1) * 128]
        wA.append(np.concatenate([q, q[:, perm], k, k[:, perm], v], axis=1).reshape(8, 128, 640))
    wA = np.ascontiguousarray(np.stack(wA))
    bg = w_in[:, 1536:2048]; cg = w_in[:, 2048:2560]; xc = w_in[:, 2560:3072]
    ga = w_in[:, 3072:4096]; gb = w_in[:, 4096:5120]
    wC = np.ascontiguousarray(np.stack([np.concatenate([bg[:, c * 128:(c + 1) * 128], cg[:, c * 128:(c + 1) * 128], xc[:, c * 128:(c + 1) * 128]], axis=1).reshape(8, 128, 384) for c in range(4)]))
    wG = np.ascontiguousarray(np.stack([np.concatenate([ga[:, c * 128:(c + 1) * 128], gb[:, c * 128:(c + 1) * 128]], axis=1).reshape(8, 128, 256) for c in range(8)]))
    sh = dict(
        ident=np.eye(128, dtype=f),
        iota256=np.ascontiguousarray(np.broadcast_to(np.arange(256, dtype=f)[None, :], (128, 256))),
        anw=np.ascontiguousarray(np.asarray(inp["attn_norm_w"], f)[0].reshape(8, 128).T),
        fnw=np.ascontiguousarray(np.asarray(inp["ffn_norm_w"], f)[0].reshape(8, 128).T),
        fnw_rep=np.ascontiguousarray(np.broadcast_to(np.asarray(inp["ffn_norm_w"], f)[0][None, :], (128, 1024))),
        finw_rep=np.ascontiguousarray(np.broadcast_to(np.asarray(inp["final_norm_w"], f)[None, :], (128, 1024))),
        subln_rep=np.ascontiguousarray(np.broadcast_to(np.asarray(inp["subln_w"], f)[0][None, :], (128, 128))),
        subcol=np.ascontiguousarray(np.asarray(inp["subln_w"], f)[0].reshape(128, 1)),
        lam_in=np.ascontiguousarray(np.broadcast_to(np.stack([np.asarray(inp[k], f)[0] for k in ("lambda_q1", "lambda_k1", "lambda_q2", "lambda_k2")])[None], (128, 4, 64))),
        convw=np.ascontiguousarray(np.asarray(inp["conv_w"], f)[0].reshape(3, 4, 128).transpose(2, 1, 0)),
        wA=wA, wC=wC, wG=wG,
        wpa=np.ascontiguousarray(np.asarray(inp["w_proj_attn"], f)[0].reshape(4, 128, 1024)),
        wpb=np.ascontiguousarray(np.asarray(inp["w_proj_conv"], f)[0].reshape(4, 128, 1024)),
        wo=np.ascontiguousarray(np.asarray(inp["w_out"], f)[0].reshape(8, 128, 1024)),
        wq=np.ascontiguousarray(np.asarray(inp["w_query"], f)[0].reshape(8, 128, 2048)),
        skT=np.ascontiguousarray(np.asarray(inp["sub_keys"], f)[0].reshape(16, 128, 128).transpose(2, 0, 1)),
        expert_u=np.ascontiguousarray(np.asarray(inp["expert_u"], f)[0]),
        expert_v=np.ascontiguousarray(np.asarray(inp["expert_v"], f)[0]),
    )
    return sh


def _rope_tables():
    inv_freq = (1.0 / (10000.0 ** (np.arange(0, 64, 2, dtype=np.float32) / np.float32(64)))).astype(np.float32)
    pos = np.arange(SEQ, dtype=np.float32)
    ang = (pos[:, None] * inv_freq[None, :]).astype(np.float32)
    d = np.arange(128) % 64
    cos = np.cos(ang)[:, d % 32].T.astype(np.float32)
    sin = np.sin(ang)[:, d % 32].T.astype(np.float32)
    sign = np.where(d < 32, -1.0, 1.0).astype(np.float32)[:, None]
    return cos, (sin * sign).astype(np.float32)


def _core_inputs(inp, sh, cos, sin, c):
    b, half = c // 2, c % 2
    x = np.asarray(inp["x"], np.float32)
    own = slice(half * T_OWN, (half + 1) * T_OWN)
    oth = slice((1 - half) * T_OWN, (2 - half) * T_OWN)
    m = dict(sh)
    m["xs"] = np.ascontiguousarray(np.concatenate([x[b, own], x[b, oth]], axis=0))
    m["cosT"] = np.ascontiguousarray(np.concatenate([cos[:, own], cos[:, oth]], axis=1))
    m["sinT"] = np.ascontiguousarray(np.concatenate([sin[:, own], sin[:, oth]], axis=1))
    fl = np.zeros((128, 2), np.float32)
    fl[:, 0] = 1.0 if half == 1 else 0.0
    fl[:, 1] = 1.0 if half == 0 else 0.0
    m["flags"] = fl
    return m


def kernel(**inputs):
    if "nc" not in _CACHE:
        _CACHE["nc"] = build_program(dbg=False)
    nc = _CACHE["nc"]
    sh = _prep_shared(inputs)
    cos, sin = _rope_tables()
    in_maps = [_core_inputs(inputs, sh, cos, sin, c) for c in range(8)]
    res = run_bass_kernel_spmd(nc, in_maps, core_ids=list(range(8)))
    out = np.empty((NB, SEQ, D_MODEL), np.float32)
    for c in range(8):
        b, half = c // 2, c % 2
        out[b, half * T_OWN:(half + 1) * T_OWN] = np.asarray(res.results[c]["out"], np.float32)
    return out
```

```python
import numpy as np
import ml_dtypes
import concourse.bass as bass
import concourse.mybir as mybir
from concourse.bass_utils import run_bass_kernel_spmd

F32 = mybir.dt.float32
BF16 = mybir.dt.bfloat16
I32 = mybir.dt.int32
U32 = mybir.dt.uint32
U8 = mybir.dt.uint8
AF = mybir.ActivationFunctionType
ALU = mybir.AluOpType
AX = mybir.AxisListType
DTSIZE = {F32: 4, BF16: 2, I32: 4, U32: 4, U8: 1}


STRICT = True


class Reg:
    __slots__ = ("name", "last_w", "readers", "disjoint")

    def __init__(self, name="", disjoint=False):
        self.name = name
        self.last_w = None
        self.readers = []
        self.disjoint = disjoint


class Eng:
    def __init__(self, fw, name, h, ndma=0):
        self.fw = fw
        self.name = name
        self.h = h
        self.sem = fw.nc.alloc_semaphore("s_" + name)
        self.count = 0
        self.waited = {}
        self.dsems = [fw.nc.alloc_semaphore("d_%s%d" % (name, i)) for i in range(ndma)]
        self.dcount = [0] * ndma
        self.drr = 0

    def wait(self, ev):
        if ev is None:
            return
        _, sem, val = ev
        k = id(sem)
        if self.waited.get(k, 0) >= val:
            return
        self.h.wait_ge(sem, val)
        self.waited[k] = val


class FW:
    def __init__(self, nc, arena_bytes=200 * 1024):
        self.nc = nc
        self.pe = Eng(self, "pe", nc.tensor)
        self.act = Eng(self, "act", nc.scalar)
        self.dve = Eng(self, "dve", nc.vector)
        self.pool = Eng(self, "pool", nc.gpsimd, ndma=12)
        self.sp = Eng(self, "sp", nc.sync, ndma=8)
        self.engs = [self.pe, self.act, self.dve, self.pool, self.sp]
        self.arena = nc.alloc_sbuf_tensor("arena", [128, arena_bytes], U8)
        self.arena_bytes = arena_bytes
        self.top = 0
        self.ps_tensors = []

    def mark(self):
        return self.top

    def release(self, m):
        self.top = m

    def alloc(self, shape, dtype, parts=128):
        n = int(np.prod(shape)) * DTSIZE[dtype]
        n_al = (n + 63) // 64 * 64
        off = self.top
        assert off + n_al <= self.arena_bytes, ("SBUF arena overflow", off, n_al)
        self.top = off + n_al
        ap = self.arena[0:parts, off:off + n].bitcast(dtype)
        if len(shape) > 1:
            names = " ".join("d%d" % i for i in range(len(shape)))
            kw = {"d%d" % i: int(s) for i, s in enumerate(shape)}
            ap = ap.rearrange("p (%s) -> p %s" % (names, names), **kw)
        return ap

    def alloc_at(self, off, shape, dtype, parts=128):
        n = int(np.prod(shape)) * DTSIZE[dtype]
        ap = self.arena[0:parts, off:off + n].bitcast(dtype)
        if len(shape) > 1:
            names = " ".join("d%d" % i for i in range(len(shape)))
            kw = {"d%d" % i: int(s) for i, s in enumerate(shape)}
            ap = ap.rearrange("p (%s) -> p %s" % (names, names), **kw)
        return ap

    def _deps(self, eng, reads, writes, is_dma=False):
        inorder = (not is_dma) and (eng.name == "pe" or (not STRICT and eng.name in ("act", "dve")))
        deps = []
        for r in reads:
            if r.last_w is not None:
                deps.append(r.last_w)
        for w in writes:
            if w.last_w is not None:
                same = (not is_dma) and w.last_w[0] == eng.name
                if not (same and (inorder or w.disjoint)):
                    deps.append(w.last_w)
            for rd in w.readers:
                if inorder and rd[0] == eng.name:
                    continue
                deps.append(rd)
        return deps

    def op(self, eng, fn, reads=(), writes=()):
        deps = self._deps(eng, reads, writes)
        for d in deps:
            if eng.name == "pe" and d[0] == "pe":
                continue
            eng.wait(d)
        ins = fn(eng.h)
        eng.count += 1
        ins.then_inc(eng.sem, 1)
        ev = (eng.name, eng.sem, eng.count)
        self._record(ev, reads, writes)
        return ev

    def _record(self, ev, reads, writes):
        for r in reads:
            r.readers = [x for x in r.readers if x[1] is not ev[1]] + [ev]
        for w in writes:
            w.last_w = ev
            w.readers = []

    def dma(self, eng, fn, reads=(), writes=()):
        deps = self._deps(eng, reads, writes, is_dma=True)
        for d in deps:
            eng.wait(d)
        i = eng.drr
        eng.drr = (i + 1) % len(eng.dsems)
        sem = eng.dsems[i]
        if eng.dcount[i] > 0:
            eng.wait(("dma", sem, eng.dcount[i]))
        ins = fn(eng.h)
        eng.dcount[i] += 16
        ins.then_inc(sem, 16)
        ev = ("dma", sem, eng.dcount[i])
        self._record(ev, reads, writes)
        return ev

    def barrier(self):
        evs = []
        for e in self.engs:
            if e.count:
                evs.append((e.name, e.sem, e.count))
            for i, s in enumerate(e.dsems):
                if e.dcount[i]:
                    evs.append(("dma", s, e.dcount[i]))
        for e in self.engs:
            for ev in evs:
                e.wait(ev)

    def finish(self, out_evs):
        for ev in out_evs:
            self.sp.wait(ev)
            self.pool.wait(ev)

D_MODEL = 1024
SEQ = 4096
NB = 4
T_OWN = 2048
EPS = 1e-6
LAMBDA_INIT = 0.8 - 0.6 * 1.0


def _regs(n, name):
    return [Reg("%s%d" % (name, i)) for i in range(n)]


RSTEPS_CFG = [3]
P4STOP = [0]


def build_program(dbg=False):
    nc = bass.Bass("TRN2", target_bir_lowering=False)

    def DI(name, shape, dt=F32):
        return nc.dram_tensor(name, list(shape), dt, kind="ExternalInput").ap()

    xs_d = DI("xs", [4096, 1024])
    cos_d = DI("cosT", [128, 4096])
    sin_d = DI("sinT", [128, 4096])
    flags_d = DI("flags", [128, 2])
    ident_d = DI("ident", [128, 128])
    iota_d = DI("iota256", [128, 256])
    anw_d = DI("anw", [128, 8])
    fnw_d = DI("fnw", [128, 8])
    fnw_rep_d = DI("fnw_rep", [128, 1024])
    finw_rep_d = DI("finw_rep", [128, 1024])
    subln_d = DI("subln_rep", [128, 128])
    subcol_d = DI("subcol", [128, 1])
    lam_d = DI("lam_in", [128, 4, 64])
    convw_d = DI("convw", [128, 4, 3])
    wA_d = DI("wA", [4, 8, 128, 640])
    wC_d = DI("wC", [4, 8, 128, 384])
    wG_d = DI("wG", [8, 8, 128, 256])
    wpa_d = DI("wpa", [4, 128, 1024])
    wpb_d = DI("wpb", [4, 128, 1024])
    wo_d = DI("wo", [8, 128, 1024])
    wq_d = DI("wq", [8, 128, 2048])
    sk_d = DI("skT", [128, 16, 128])
    eu_d = DI("expert_u", [16384, 1024])
    ev_d = DI("expert_v", [16384, 1024])
    uvs_d = nc.dram_tensor("uvs", [16384, 2048], BF16, kind="Internal").ap()
    out_d = nc.dram_tensor("out", [T_OWN, 1024], F32, kind="ExternalOutput").ap()
    dbg_d = {}
    if dbg:
        dbg_d["attnT"] = nc.dram_tensor("dbg_attnT", [128, 4, 2048], F32, kind="ExternalOutput").ap()
        dbg_d["h"] = nc.dram_tensor("dbg_h", [128, 16, 1024], F32, kind="ExternalOutput").ap()
        dbg_d["eidx"] = nc.dram_tensor("dbg_eidx", [128, 128], I32, kind="ExternalOutput").ap()
        dbg_d["g"] = nc.dram_tensor("dbg_g", [128, 128], F32, kind="ExternalOutput").ap()
        dbg_d["xnT"] = nc.dram_tensor("dbg_xnT", [128, 8, 2048], F32, kind="ExternalOutput").ap()
        dbg_d["uvs"] = nc.dram_tensor("dbg_uvs", [3, 128, 2048], BF16, kind="ExternalOutput").ap()

    fw = FW(nc, arena_bytes=207 * 1024)
    sp, pool, act, dve, pe = fw.sp, fw.pool, fw.act, fw.dve, fw.pe
    ps = [nc.alloc_psum_tensor("ps%d" % i, [128, 512], F32) for i in range(6)]
    pt = [nc.alloc_psum_tensor("pt%d" % i, [128, 1024], BF16) for i in range(2)]
    PS = _regs(6, "ps")
    PT = _regs(2, "pt")
    outs = []

    def dma_in(dst, src, reg, eng=None):
        return fw.dma(eng or sp, lambda e: e.dma_start(out=dst, in_=src), writes=[reg])

    def dbg_dump(name, src_ap, reg, shape):
        if not dbg:
            return
        dst = dbg_d[name]
        outs.append(fw.dma(sp, lambda e: e.dma_start(out=dst, in_=src_ap), reads=[reg]))

    ident_f = fw.alloc([128], F32); ident = fw.alloc([128], BF16)
    anw = fw.alloc([8], F32); fnw = fw.alloc([8], F32)
    subln = fw.alloc([128], F32); flags = fw.alloc([2], F32)
    lam_in = fw.alloc([4, 64], F32); lam_t = fw.alloc([8], F32); lam_j = fw.alloc([64], F32)
    convw = fw.alloc([4, 3], F32)
    RC = Reg("consts")
    for dst, src in ((ident_f, ident_d), (anw, anw_d), (fnw, fnw_d), (subln, subln_d), (flags, flags_d),
                     (lam_in, lam_d), (convw, convw_d)):
        dma_in(dst, src, RC)
    RCI = Reg("ident")
    fw.barrier()
    fw.op(dve, lambda e: e.tensor_copy(out=ident, in_=ident_f), reads=[RC], writes=[RCI])
    fw.op(dve, lambda e: e.tensor_scalar(out=subln, in0=subln, scalar1=1.0 - LAMBDA_INIT, scalar2=None, op0=ALU.mult), reads=[RC], writes=[RC])
    fw.op(dve, lambda e: e.scalar_tensor_tensor(out=lam_j, in0=lam_in[:, 0, :], scalar=1.0, in1=lam_in[:, 1, :], op0=ALU.mult, op1=ALU.mult, accum_out=lam_t[:, 0:1]), reads=[RC], writes=[RC])
    fw.op(dve, lambda e: e.scalar_tensor_tensor(out=lam_j, in0=lam_in[:, 2, :], scalar=1.0, in1=lam_in[:, 3, :], op0=ALU.mult, op1=ALU.mult, accum_out=lam_t[:, 1:2]), reads=[RC], writes=[RC])
    fw.op(act, lambda e: e.activation(out=lam_t[:, 2:4], in_=lam_t[:, 0:2], func=AF.Exp), reads=[RC], writes=[RC])
    fw.op(dve, lambda e: e.tensor_tensor(out=lam_t[:, 4:5], in0=lam_t[:, 3:4], in1=lam_t[:, 2:3], op=ALU.subtract), reads=[RC], writes=[RC])
    fw.op(dve, lambda e: e.tensor_scalar(out=lam_t[:, 4:5], in0=lam_t[:, 4:5], scalar1=-LAMBDA_INIT, scalar2=None, op0=ALU.add), reads=[RC], writes=[RC])
    neglam = lam_t[:, 4:5]

    off_own = fw.top
    xnT_own = fw.alloc([8, 2048], BF16)
    attnT = fw.alloc([4, 2048], BF16)
    ucT = fw.alloc([4, 2048], BF16)
    off_oth = fw.top
    xnT_oth = fw.alloc([8, 2048], BF16)
    m_pers = fw.mark()
    RXN = [Reg("xnT%d" % i, disjoint=True) for i in range(32)]
    stat = [fw.alloc([4], F32) for _ in range(4)]
    RST = _regs(4, "stat")
    m_tmp = fw.mark()

    def rms_rstd(src_ap, n, st, rst, junk, rjunk, src_regs):
        fw.op(act, lambda e: e.activation(out=junk, in_=src_ap, func=AF.Square, accum_out=st[:, 0:1]), reads=src_regs, writes=[rjunk, rst])
        fw.op(dve, lambda e: e.tensor_scalar(out=st[:, 1:2], in0=st[:, 0:1], scalar1=1.0 / n, scalar2=EPS, op0=ALU.mult, op1=ALU.add), reads=[rst], writes=[rst])
        fw.op(act, lambda e: e.activation(out=st[:, 3:4], in_=st[:, 1:2], func=AF.Ln), reads=[rst], writes=[rst])
        fw.op(act, lambda e: e.activation(out=st[:, 2:3], in_=st[:, 3:4], func=AF.Exp, scale=-0.5), reads=[rst], writes=[rst])

    def norm_transpose(load_fn, nblk, dst_fn, wcol, regs_dst, post_fn=None):
        xbuf = [fw.alloc([1024], F32) for _ in range(4)]; RX = _regs(4, "xbuf")
        xnb = [fw.alloc([1024], BF16) for _ in range(2)]; RXB = _regs(2, "xnb")
        junk = fw.alloc([1024], BF16); RJ = Reg("junk")
        srcs = {}

        def stage_a(blk):
            xs = xbuf[blk % 4]; rx = RX[blk % 4]
            src_ap, src_regs = load_fn(blk, xs, rx)
            srcs[blk] = (src_ap, src_regs)
            st = stat[blk % 4]; rst = RST[blk % 4]
            fw.op(act, lambda e: e.activation(out=junk, in_=src_ap, func=AF.Square, accum_out=st[:, 0:1]), reads=src_regs, writes=[RJ, rst])
            fw.op(dve, lambda e: e.tensor_scalar(out=st[:, 1:2], in0=st[:, 0:1], scalar1=1.0 / 1024, scalar2=EPS, op0=ALU.mult, op1=ALU.add), reads=[rst], writes=[rst])

        def stage_b(blk):
            src_ap, src_regs = srcs.pop(blk)
            st = stat[blk % 4]; rst = RST[blk % 4]
            fw.op(act, lambda e: e.activation(out=st[:, 3:4], in_=st[:, 1:2], func=AF.Ln), reads=[rst], writes=[rst])
            fw.op(act, lambda e: e.activation(out=st[:, 2:3], in_=st[:, 3:4], func=AF.Exp, scale=-0.5), reads=[rst], writes=[rst])
            if post_fn is not None:
                post_fn(blk, st, rst)
            xn = xnb[blk % 2]; rxn = RXB[blk % 2]
            fw.op(act, lambda e: e.activation(out=xn, in_=src_ap, func=AF.Copy, scale=st[:, 2:3]), reads=src_regs + [rst], writes=[rxn])
            p = pt[blk % 2]; rp = PT[blk % 2]
            for kc in range(8):
                fw.op(pe, lambda e: e.transpose(out=p[:, kc * 128:(kc + 1) * 128], in_=xn[:, kc * 128:(kc + 1) * 128], identity=ident), reads=[rxn, RCI], writes=[rp])
            for kc in range(8):
                fw.op(dve, lambda e: e.tensor_scalar(out=dst_fn(blk, kc), in0=p[:, kc * 128:(kc + 1) * 128], scalar1=wcol[:, kc:kc + 1], scalar2=None, op0=ALU.mult), reads=[rp, RC], writes=[regs_dst[blk]])

        stage_a(0)
        for blk in range(nblk):
            if blk + 1 < nblk:
                stage_a(blk + 1)
            stage_b(blk)

    def load_x(blk, xs, rx):
        dma_in(xs, xs_d[blk * 128:(blk + 1) * 128, :], rx)
        return xs, [rx]

    def xn_dst(blk, kc):
        if blk < 16:
            return xnT_own[:, kc, blk * 128:(blk + 1) * 128]
        return xnT_oth[:, kc, (blk - 16) * 128:(blk - 15) * 128]

    norm_transpose(load_x, 32, xn_dst, anw, RXN)
    fw.release(m_tmp)
    fw.barrier()

    def xt_tile(t, kc):
        if t < 4:
            return xnT_own[:, kc, t * 512:(t + 1) * 512]
        return xnT_oth[:, kc, (t - 4) * 512:(t - 3) * 512]

    def xt_regs(t):
        return RXN[t * 4:(t + 1) * 4]

    cosT = fw.alloc([4096], F32); sinT = fw.alloc([4096], F32); RTAB = Reg("tab")
    dma_in(cosT, cos_d, RTAB); dma_in(sinT, sin_d, RTAB)
    subcol = fw.alloc([1], F32)
    dma_in(subcol, subcol_d, RC)
    ones_bf = fw.alloc([128], BF16); RONE = Reg("ones")
    fw.barrier()
    fw.op(dve, lambda e: e.tensor_scalar(out=subcol, in0=subcol, scalar1=1.0 - LAMBDA_INIT, scalar2=None, op0=ALU.mult), reads=[RC], writes=[RC])
    fw.op(pool, lambda e: e.memset(ones_bf, 1.0), writes=[RONE])
    wab = [fw.alloc([8, 640], BF16) for _ in range(2)]; RWAb = [_regs(8, "wa%d" % i) for i in range(2)]

    def load_wa(hd):
        for kc in range(8):
            fw.dma(pool, lambda e: e.dma_start(out=wab[hd % 2][:, kc, :], in_=wA_d[hd, kc]), writes=[RWAb[hd % 2][kc]])
    kT = fw.alloc([4096], BF16); RK = Reg("kT")
    qT = fw.alloc([2048], BF16); RQ = Reg("qT")
    vx = fw.alloc([4096], BF16); RV = Reg("vx")
    t1b = [fw.alloc([512], F32) for _ in range(2)]; RT1 = _regs(2, "t1")
    t2b = [fw.alloc([512], F32) for _ in range(2)]; RT2 = _regs(2, "t2")
    pTb = [fw.alloc([1024], BF16) for _ in range(3)]; RPT = _regs(3, "pT")
    accp = fw.alloc([1024], F32); RACP = Reg("accp")
    rz = fw.alloc([1024], F32); RRZ = Reg("rz")
    RAT = Reg("attnT")
    ropei = [0]
    cvt = fw.alloc([2, 2048], BF16)
    eu_v = eu_d.rearrange("(c p j) d -> c p j d", p=128, j=2)
    ev_v = ev_d.rearrange("(c p j) d -> c p j d", p=128, j=2)
    uvs_v = uvs_d.rearrange("(c p j) d -> c p j d", p=128, j=2)
    RCVu = Reg("cvtu"); RCVv = Reg("cvtv")

    def convert_chunk(c):
        fw.dma(pool, lambda e: e.dma_start(out=cvt[:, :, 0:1024], in_=eu_v[c]), writes=[RCVu])
        fw.dma(pool, lambda e: e.dma_start(out=cvt[:, :, 1024:2048], in_=ev_v[c]), writes=[RCVv])
        fw.dma(sp, lambda e: e.dma_start(out=uvs_v[c], in_=cvt), reads=[RCVu, RCVv])

    def rope(bank_a, ra, bank_b, rb, t, dst, rdst):
        i = ropei[0] % 2; ropei[0] += 1
        fw.op(dve, lambda e: e.tensor_tensor(out=t1b[i], in0=bank_a[:, 0:512], in1=cosT[:, t * 512:(t + 1) * 512], op=ALU.mult), reads=[ra, RTAB], writes=[RT1[i]])
        fw.op(dve, lambda e: e.tensor_tensor(out=t2b[i], in0=bank_b[:, 0:512], in1=sinT[:, t * 512:(t + 1) * 512], op=ALU.mult), reads=[rb, RTAB], writes=[RT2[i]])
        fw.op(pool, lambda e: e.tensor_tensor(out=dst, in0=t1b[i], in1=t2b[i], op=ALU.add), reads=[RT1[i], RT2[i]], writes=[rdst])

    cvi = [0]
    load_wa(0)
    for h in range(4):
        wa = wab[h % 2]; RWA = RWAb[h % 2]
        if h + 1 < 4:
            load_wa(h + 1)
        for t in range(8):
            groups = [(256, 0), (384, 1)] + ([(0, 2), (128, 3)] if t < 4 else [])
            for col0, bnk in groups:
                for kc in range(8):
                    fw.op(pe, lambda e: e.matmul(out=ps[bnk][:, 0:512], lhsT=wa[:, kc, col0:col0 + 128], rhs=xt_tile(t, kc), start=(kc == 0), stop=(kc == 7)), reads=[RWA[kc]] + xt_regs(t), writes=[PS[bnk]])
            rope(ps[0], PS[0], ps[1], PS[1], t, kT[:, t * 512:(t + 1) * 512], RK)
            if t < 4:
                rope(ps[2], PS[2], ps[3], PS[3], t, qT[:, t * 512:(t + 1) * 512], RQ)
            for j in range(4):
                for kc in range(8):
                    fw.op(pe, lambda e: e.matmul(out=ps[4][:, j * 128:(j + 1) * 128], lhsT=xt_tile(t, kc)[:, j * 128:(j + 1) * 128], rhs=wa[:, kc, 512:640], start=(kc == 0), stop=(kc == 7)), reads=[RWA[kc]] + xt_regs(t), writes=[PS[4]])
            fw.op(act, lambda e: e.activation(out=vx[:, t * 512:(t + 1) * 512], in_=ps[4][:, 0:512], func=AF.Copy), reads=[PS[4]], writes=[RV])
        for qt in range(4):
            q0 = qt * 512
            for _ in range(4):
                convert_chunk(cvi[0]); cvi[0] += 1

            def QK(kb):
                for m in range(2):
                    bnk = (kb % 2) * 2 + m
                    fw.op(pe, lambda e: e.matmul(out=ps[bnk][:, 0:512], lhsT=kT[m * 64:(m + 1) * 64, kb * 128:(kb + 1) * 128], rhs=qT[m * 64:(m + 1) * 64, q0:q0 + 512], start=True, stop=True), reads=[RK, RQ], writes=[PS[bnk]])

            def EXP(kb):
                for m in range(2):
                    bnk = (kb % 2) * 2 + m
                    fw.op(act, lambda e: e.activation(out=pTb[kb % 3][:, m * 512:(m + 1) * 512], in_=ps[bnk][:, 0:512], func=AF.Exp, scale=0.125), reads=[PS[bnk]], writes=[RPT[kb % 3]])

            def PV(kb):
                for m in range(2):
                    fw.op(pe, lambda e: e.matmul(out=ps[4 + m][:, 0:512], lhsT=vx[:, kb * 128:(kb + 1) * 128], rhs=pTb[kb % 3][:, m * 512:(m + 1) * 512], start=(kb == 0), stop=(kb == 31)), reads=[RPT[kb % 3], RV], writes=[PS[4 + m]])
                if kb == 0:
                    fw.op(dve, lambda e: e.tensor_copy(out=accp, in_=pTb[kb % 3]), reads=[RPT[kb % 3]], writes=[RACP])
                else:
                    fw.op(dve, lambda e: e.tensor_tensor(out=accp, in0=accp, in1=pTb[kb % 3], op=ALU.add), reads=[RPT[kb % 3], RACP], writes=[RACP])

            for kb in range(33):
                if kb < 32:
                    QK(kb); EXP(kb)
                if kb >= 1:
                    PV(kb - 1)
            hi, lo = pTb[0], pTb[1]
            fw.op(dve, lambda e: e.tensor_copy(out=hi, in_=accp), reads=[RACP], writes=[RPT[0]])
            fw.op(dve, lambda e: e.tensor_tensor(out=lo, in0=accp, in1=hi, op=ALU.subtract), reads=[RACP, RPT[0]], writes=[RPT[1]])
            for m in range(2):
                fw.op(pe, lambda e: e.matmul(out=ps[m][:, 0:512], lhsT=ones_bf, rhs=hi[:, m * 512:(m + 1) * 512], start=True, stop=False), reads=[RONE, RPT[0]], writes=[PS[m]])
                fw.op(pe, lambda e: e.matmul(out=ps[m][:, 0:512], lhsT=ones_bf, rhs=lo[:, m * 512:(m + 1) * 512], start=False, stop=True), reads=[RONE, RPT[1]], writes=[PS[m]])
                fw.op(dve, lambda e: e.reciprocal(out=rz[:, m * 512:(m + 1) * 512], in_=ps[m][:, 0:512]), reads=[PS[m]], writes=[RRZ])
            ta, tb2, tc, td = t1b[0], t2b[0], t1b[1], t2b[1]
            rta, rtb, rtc, rtd = RT1[0], RT2[0], RT1[1], RT2[1]
            fw.op(dve, lambda e: e.tensor_tensor(out=ta, in0=ps[4][:, 0:512], in1=rz[:, 0:512], op=ALU.mult), reads=[PS[4], RRZ], writes=[rta])
            fw.op(dve, lambda e: e.tensor_tensor(out=tb2, in0=ps[5][:, 0:512], in1=rz[:, 512:1024], op=ALU.mult), reads=[PS[5], RRZ], writes=[rtb])
            fw.op(dve, lambda e: e.scalar_tensor_tensor(out=tc, in0=tb2, scalar=neglam, in1=ta, op0=ALU.mult, op1=ALU.add), reads=[rtb, rta, RC], writes=[rtc])
            sq = pTb[2][:, 0:512]
            fw.op(act, lambda e: e.activation(out=sq, in_=tc, func=AF.Square), reads=[rtc], writes=[RPT[2]])
            fw.op(pe, lambda e: e.matmul(out=ps[2][:, 0:512], lhsT=ones_bf, rhs=sq, start=True, stop=True), reads=[RONE, RPT[2]], writes=[PS[2]])
            fw.op(dve, lambda e: e.tensor_scalar(out=td, in0=ps[2][:, 0:512], scalar1=1.0 / 128, scalar2=EPS, op0=ALU.mult, op1=ALU.add), reads=[PS[2]], writes=[rtd])
            fw.op(act, lambda e: e.activation(out=td, in_=td, func=AF.Ln), reads=[rtd], writes=[rtd])
            fw.op(act, lambda e: e.activation(out=td, in_=td, func=AF.Exp, scale=-0.5), reads=[rtd], writes=[rtd])
            fw.op(dve, lambda e: e.scalar_tensor_tensor(out=attnT[:, h, q0:q0 + 512], in0=tc, scalar=subcol[:, 0:1], in1=td, op0=ALU.mult, op1=ALU.mult), reads=[rtc, rtd, RC], writes=[RAT])
    fw.barrier()
    if dbg:
        stg = fw.alloc([2048], F32); RSTG = Reg("stg")
        for hh in range(4):
            fw.op(dve, lambda e: e.tensor_copy(out=stg, in_=attnT[:, hh, :]), reads=[RAT], writes=[RSTG])
            outs.append(fw.dma(sp, lambda e: e.dma_start(out=dbg_d["attnT"][:, hh, :], in_=stg), reads=[RSTG]))
        for kc in range(8):
            fw.op(dve, lambda e: e.tensor_copy(out=stg, in_=xnT_own[:, kc, :]), reads=RXN[0:16], writes=[RSTG])
            outs.append(fw.dma(sp, lambda e: e.dma_start(out=dbg_d["xnT"][:, kc, :], in_=stg), reads=[RSTG]))
        fw.barrier()
        stb = fw.alloc([2048], BF16); RSTB = Reg("stb")
        for i, r0 in enumerate((0, 640, 16256)):
            fw.dma(sp, lambda e: e.dma_start(out=stb, in_=uvs_d[r0:r0 + 128, :]), writes=[RSTB])
            outs.append(fw.dma(sp, lambda e: e.dma_start(out=dbg_d["uvs"][i], in_=stb), reads=[RSTB]))
        fw.barrier()
    fw.release(m_tmp)

    wcb = [fw.alloc([8, 384], BF16) for _ in range(2)]; RWCb = [_regs(8, "wc%d" % i) for i in range(2)]

    def load_wc(cc):
        for kc in range(8):
            fw.dma(pool, lambda e: e.dma_start(out=wcb[cc % 2][:, kc, :], in_=wC_d[cc, kc]), writes=[RWCb[cc % 2][kc]])

    zT = fw.alloc([2050], F32); RZ = Reg("zT")
    bgs = fw.alloc([2048], F32); RBG = Reg("bgs")
    tmpc = [fw.alloc([512], F32) for _ in range(2)]; RTC = _regs(2, "tmpc")
    c1 = fw.alloc([2048], F32); RC1 = Reg("c1"); c2 = fw.alloc([2048], F32); RC2 = Reg("c2")
    hs = fw.alloc([16], F32); RHS = Reg("hs")
    RUC = Reg("ucT")
    load_wc(0)
    for cc in range(4):
        wc = wcb[cc % 2]; RWC = RWCb[cc % 2]
        if cc + 1 < 4:
            load_wc(cc + 1)
        for t in range(4):
            for gi, col0 in enumerate((128, 256, 0)):
                for kc in range(8):
                    fw.op(pe, lambda e: e.matmul(out=ps[gi][:, 0:512], lhsT=wc[:, kc, col0:col0 + 128], rhs=xt_tile(t, kc), start=(kc == 0), stop=(kc == 7)), reads=[RWC[kc]] + xt_regs(t), writes=[PS[gi]])
            fw.op(act, lambda e: e.activation(out=tmpc[t % 2], in_=ps[0][:, 0:512], func=AF.Copy), reads=[PS[0]], writes=[RTC[t % 2]])
            fw.op(dve, lambda e: e.tensor_tensor(out=zT[:, 1 + t * 512:1 + (t + 1) * 512], in0=ps[1][:, 0:512], in1=tmpc[t % 2], op=ALU.mult), reads=[PS[1], RTC[t % 2]], writes=[RZ])
            fw.op(act, lambda e: e.activation(out=bgs[:, t * 512:(t + 1) * 512], in_=ps[2][:, 0:512], func=AF.Copy), reads=[PS[2]], writes=[RBG])
        for gi, (col0, tok) in enumerate(((128, 0), (256, 0), (128, 2044), (256, 2044))):
            for kc in range(8):
                fw.op(pe, lambda e: e.matmul(out=ps[3][:, gi * 4:gi * 4 + 4], lhsT=wc[:, kc, col0:col0 + 128], rhs=xnT_oth[:, kc, tok:tok + 4], start=(kc == 0), stop=(kc == 7)), reads=[RWC[kc]] + RXN[16:32], writes=[PS[3]])
        fw.op(dve, lambda e: e.tensor_copy(out=hs, in_=ps[3][:, 0:16]), reads=[PS[3]], writes=[RHS])
        fw.op(dve, lambda e: e.scalar_tensor_tensor(out=zT[:, 2049:2050], in0=hs[:, 0:1], scalar=flags[:, 1:2], in1=hs[:, 4:5], op0=ALU.mult, op1=ALU.mult), reads=[RHS, RC], writes=[RZ])
        fw.op(dve, lambda e: e.scalar_tensor_tensor(out=zT[:, 0:1], in0=hs[:, 11:12], scalar=flags[:, 0:1], in1=hs[:, 15:16], op0=ALU.mult, op1=ALU.mult), reads=[RHS, RC], writes=[RZ])
        fw.op(dve, lambda e: e.tensor_scalar(out=c1, in0=zT[:, 0:2048], scalar1=convw[:, cc, 0:1], scalar2=None, op0=ALU.mult), reads=[RZ, RC], writes=[RC1])
        fw.op(dve, lambda e: e.scalar_tensor_tensor(out=c2, in0=zT[:, 1:2049], scalar=convw[:, cc, 1:2], in1=c1, op0=ALU.mult, op1=ALU.add), reads=[RZ, RC, RC1], writes=[RC2])
        fw.op(dve, lambda e: e.scalar_tensor_tensor(out=c1, in0=zT[:, 2:2050], scalar=convw[:, cc, 2:3], in1=c2, op0=ALU.mult, op1=ALU.add), reads=[RZ, RC, RC2], writes=[RC1])
        fw.op(dve, lambda e: e.tensor_tensor(out=ucT[:, cc, :], in0=c1, in1=bgs, op=ALU.mult), reads=[RC1, RBG], writes=[RUC])
    fw.barrier()
    fw.release(m_tmp)

    mergedT = xnT_oth; RMG = Reg("merged")
    wpa_sb = fw.alloc([4, 1024], BF16); wpb_sb = fw.alloc([4, 1024], BF16); RWP = _regs(8, "wp")
    for c4 in range(4):
        fw.dma(pool, lambda e: e.dma_start(out=wpa_sb[:, c4, :], in_=wpa_d[c4]), writes=[RWP[c4]])
        fw.dma(pool, lambda e: e.dma_start(out=wpb_sb[:, c4, :], in_=wpb_d[c4]), writes=[RWP[4 + c4]])
    wgb = [fw.alloc([8, 256], BF16) for _ in range(2)]; RWGb = [_regs(8, "wg%d" % i) for i in range(2)]

    def load_wg(dc):
        for kc in range(8):
            fw.dma(pool, lambda e: e.dma_start(out=wgb[dc % 2][:, kc, :], in_=wG_d[dc, kc]), writes=[RWGb[dc % 2][kc]])

    sgAb = [fw.alloc([512], F32) for _ in range(2)]; sgBb = [fw.alloc([512], F32) for _ in range(2)]; RSGb = [_regs(2, "sg%d" % i) for i in range(2)]
    m1b = [fw.alloc([512], F32) for _ in range(2)]; m2b = [fw.alloc([512], F32) for _ in range(2)]; RMb = [_regs(2, "m%d" % i) for i in range(2)]
    load_wg(0)
    for dc in range(8):
        wg = wgb[dc % 2]; RWG = RWGb[dc % 2]
        if dc + 1 < 8:
            load_wg(dc + 1)
        for t in range(4):
            ba, bc = (0, 1) if t % 2 == 0 else (4, 5)
            sgA, sgB, RSG = sgAb[t % 2], sgBb[t % 2], RSGb[t % 2]
            m1, m2, RM = m1b[t % 2], m2b[t % 2], RMb[t % 2]
            for c4 in range(4):
                fw.op(pe, lambda e: e.matmul(out=ps[ba][:, 0:512], lhsT=wpa_sb[:, c4, dc * 128:(dc + 1) * 128], rhs=attnT[:, c4, t * 512:(t + 1) * 512], start=(c4 == 0), stop=(c4 == 3)), reads=[RWP[c4], RAT], writes=[PS[ba]])
            for c4 in range(4):
                fw.op(pe, lambda e: e.matmul(out=ps[bc][:, 0:512], lhsT=wpb_sb[:, c4, dc * 128:(dc + 1) * 128], rhs=ucT[:, c4, t * 512:(t + 1) * 512], start=(c4 == 0), stop=(c4 == 3)), reads=[RWP[4 + c4], RUC], writes=[PS[bc]])
            for gi in range(2):
                for kc in range(8):
                    fw.op(pe, lambda e: e.matmul(out=ps[2 + gi][:, 0:512], lhsT=wg[:, kc, gi * 128:(gi + 1) * 128], rhs=xt_tile(t, kc), start=(kc == 0), stop=(kc == 7)), reads=[RWG[kc]] + xt_regs(t), writes=[PS[2 + gi]])
            fw.op(act, lambda e: e.activation(out=sgA, in_=ps[2][:, 0:512], func=AF.Sigmoid), reads=[PS[2]], writes=[RSG[0]])
            fw.op(act, lambda e: e.activation(out=sgB, in_=ps[3][:, 0:512], func=AF.Sigmoid), reads=[PS[3]], writes=[RSG[1]])
            fw.op(dve, lambda e: e.tensor_tensor(out=m1, in0=ps[ba][:, 0:512], in1=sgA, op=ALU.mult), reads=[PS[ba], RSG[0]], writes=[RM[0]])
            fw.op(dve, lambda e: e.tensor_tensor(out=m2, in0=ps[bc][:, 0:512], in1=sgB, op=ALU.mult), reads=[PS[bc], RSG[1]], writes=[RM[1]])
            fw.op(pool, lambda e: e.tensor_tensor(out=mergedT[:, dc, t * 512:(t + 1) * 512], in0=m1, in1=m2, op=ALU.add), reads=[RM[0], RM[1]], writes=[RMG])
    fw.barrier()
    fw.release(m_tmp)

    hT = fw.alloc_at(off_own, [16, 1024], F32); RH = _regs(16, "h")
    wo_sb = fw.alloc([8, 1024], BF16); RWO = _regs(8, "wo")
    for dc in range(8):
        fw.dma(pool, lambda e: e.dma_start(out=wo_sb[:, dc, :], in_=wo_d[dc]), writes=[RWO[dc]])
    xres = [fw.alloc([1024], F32) for _ in range(2)]; RXR = _regs(2, "xres")
    for blk in range(16):
        dma_in(xres[blk % 2], xs_d[blk * 128:(blk + 1) * 128, :], RXR[blk % 2])
        for half in range(2):
            for dc in range(8):
                fw.op(pe, lambda e: e.matmul(out=ps[half][:, 0:512], lhsT=mergedT[:, dc, blk * 128:(blk + 1) * 128], rhs=wo_sb[:, dc, half * 512:(half + 1) * 512], start=(dc == 0), stop=(dc == 7)), reads=[RMG, RWO[dc]], writes=[PS[half]])
            fw.op(dve, lambda e: e.tensor_tensor(out=hT[:, blk, half * 512:(half + 1) * 512], in0=ps[half][:, 0:512], in1=xres[blk % 2][:, half * 512:(half + 1) * 512], op=ALU.add), reads=[PS[half], RXR[blk % 2]], writes=[RH[blk]])
    fw.barrier()
    fw.release(m_tmp)
    if dbg:
        for blk in range(16):
            outs.append(fw.dma(sp, lambda e: e.dma_start(out=dbg_d["h"][:, blk, :], in_=hT[:, blk, :]), reads=[RH[blk]]))

    wq_sb = fw.alloc_at(off_oth, [8, 2048], BF16); RWQs = _regs(16, "wq")
    for kc in range(8):
        for hf in range(2):
            fw.dma(pool, lambda e: e.dma_start(out=wq_sb[:, kc, hf * 1024:(hf + 1) * 1024], in_=wq_d[kc][:, hf * 1024:(hf + 1) * 1024]), writes=[RWQs[kc * 2 + hf]])
    rstd_all = fw.alloc([16], F32); ss_all = fw.alloc([16], F32); RRS = Reg("rstd_all")
    NR = 4
    eidx_r = [fw.alloc([128], I32) for _ in range(NR)]; REA = _regs(NR, "eidx_r")
    g_r = [fw.alloc([128], F32) for _ in range(NR)]; RGA = _regs(NR, "g_r")
    iota256 = fw.alloc([64], F32)
    fnw_rep = fw.alloc([1024], F32); finw_rep = fw.alloc([1024], F32)
    scs = fw.alloc([2048], F32); RSC = Reg("scs")
    sk_f = scs.rearrange("p (a b) -> p a b", a=16, b=128); sk_b = fw.alloc([16, 128], BF16); RSK = Reg("sk")
    dma_in(iota256, iota_d[:, 0:64], RC); dma_in(fnw_rep, fnw_rep_d, RC); dma_in(finw_rep, finw_rep_d, RC)
    dma_in(sk_f, sk_d, RSK)
    fw.barrier()
    fw.op(dve, lambda e: e.tensor_copy(out=sk_b, in_=sk_f), reads=[RSK], writes=[RSK])
    junkn = fw.alloc([1024], BF16); RJN = Reg("junkn")
    for blk in range(16):
        fw.op(act, lambda e: e.activation(out=junkn, in_=hT[:, blk, :], func=AF.Square, accum_out=ss_all[:, blk:blk + 1]), reads=[RH[blk]], writes=[RJN, RRS])
    fw.op(dve, lambda e: e.tensor_scalar(out=ss_all, in0=ss_all, scalar1=1.0 / 1024, scalar2=EPS, op0=ALU.mult, op1=ALU.add), reads=[RRS], writes=[RRS])
    fw.op(act, lambda e: e.activation(out=ss_all, in_=ss_all, func=AF.Ln), reads=[RRS], writes=[RRS])
    fw.op(act, lambda e: e.activation(out=rstd_all, in_=ss_all, func=AF.Exp, scale=-0.5), reads=[RRS], writes=[RRS])
    fw.barrier()

    if P4STOP[0] == 1:
        fw.barrier(); fw.finish(outs); return nc
    RECTS = [(0, 2, 16, 0), (2, 2, 5, 32), (4, 4, 3, 42), (8, 8, 1, 54)]
    NCAND = 62
    qps = fw.alloc([2048], BF16); RQP = Reg("qps")
    top = fw.alloc([256], F32); RTOPs = [Reg("top%d" % i, disjoint=True) for i in range(16)]
    idxu = fw.alloc([256], U32); RIDXs = [Reg("idxu%d" % i, disjoint=True) for i in range(16)]
    workb = [fw.alloc([128], F32) for _ in range(2)]; RWKb = _regs(2, "workb")
    HS = [dict(cand=fw.alloc([NCAND], F32), cidx=fw.alloc([NCAND], F32), work2=fw.alloc([NCAND], F32),
               posu=fw.alloc([16], U32), posf=fw.alloc([16], F32),
               RCD=Reg("cand", disjoint=True), RCI=Reg("cidx", disjoint=True), RWK2=Reg("work2"),
               RPOS=Reg("posu", disjoint=True), RPOSF=Reg("posf")) for _ in range(2)]
    RCTs = [Reg("ctop%d" % i, disjoint=True) for i in range(8)]
    idxf = fw.alloc([256], F32); idxf128 = fw.alloc([256], F32); RIF = Reg("idxf")
    ctop = fw.alloc([128], F32); RCT = Reg("ctop", disjoint=True)
    NJ = 4
    junkc = [fw.alloc([NCAND], F32) for _ in range(NJ)]; RJ2 = _regs(NJ, "junkc")
    eidxf = fw.alloc([128], F32); REI = Reg("eidxf", disjoint=True)
    posu = fw.alloc([16], U32); RPOS = Reg("posu", disjoint=True); posf = fw.alloc([16], F32); RPOSF = Reg("posf")
    negmax = fw.alloc([8], F32); gs = fw.alloc([8], F32); rg = fw.alloc([8], F32); RSM = Reg("sm", disjoint=True)
    xnb = [fw.alloc([1024], BF16) for _ in range(1)]; RXB = _regs(1, "xnb")
    hnb = [fw.alloc([8, 128], BF16) for _ in range(2)]; RHB = [_regs(8, "hnb%d" % i) for i in range(2)]
    jc = [0]

    def route_pre(blk):
        xn = xnb[0]; rxn = RXB[0]
        hb = hnb[blk % 2]; rhb = RHB[blk % 2]
        fw.op(act, lambda e: e.activation(out=xn, in_=hT[:, blk, :], func=AF.Copy, scale=rstd_all[:, blk:blk + 1]), reads=[RH[blk], RRS], writes=[rxn])
        p = pt[blk % 2]; rp = PT[blk % 2]
        for kc in range(8):
            fw.op(pe, lambda e: e.transpose(out=p[:, kc * 128:(kc + 1) * 128], in_=xn[:, kc * 128:(kc + 1) * 128], identity=ident), reads=[rxn, RCI], writes=[rp])
        for kc in range(8):
            fw.op(act, lambda e: e.activation(out=hb[:, kc, :], in_=p[:, kc * 128:(kc + 1) * 128], func=AF.Copy, scale=fnw[:, kc:kc + 1]), reads=[rp, RC], writes=[rhb[kc]])

    def route(blk):
        slot = blk % NR
        gt = g_r[slot]; RG = RGA[slot]
        hb = hnb[blk % 2]; rhb = RHB[blk % 2]
        for g in range(4):
            bank = ps[4 + g % 2]; rb = PS[4 + g % 2]
            for j in range(4):
                hp = g * 4 + j
                for kc in range(8):
                    fw.op(pe, lambda e: e.matmul(out=bank[:, j * 128:(j + 1) * 128], lhsT=wq_sb[:, kc, hp * 128:(hp + 1) * 128], rhs=hb[:, kc, :], start=(kc == 0), stop=(kc == 7)), reads=[RWQs[kc * 2 + hp // 8], rhb[kc]], writes=[rb])
            fw.op(act, lambda e: e.activation(out=qps[:, g * 512:(g + 1) * 512], in_=bank[:, 0:512], func=AF.Copy), reads=[rb], writes=[RQP])
        yield
        for g in range(4):
            bank = ps[4 + g % 2]; rb = PS[4 + g % 2]
            for j in range(4):
                hp = g * 4 + j
                fw.op(pe, lambda e: e.matmul(out=bank[:, j * 128:(j + 1) * 128], lhsT=qps[:, hp * 128:(hp + 1) * 128], rhs=sk_b[:, hp, :], start=True, stop=True), reads=[RQP, RSK], writes=[rb])
            fw.op(act, lambda e: e.activation(out=scs[:, g * 512:(g + 1) * 512], in_=bank[:, 0:512], func=AF.Copy), reads=[rb], writes=[RSC])
        yield
        def topk_chain(hp):
            sv = scs[:, hp * 128:(hp + 1) * 128]
            wk = workb[hp % 2]; rwk = RWKb[hp % 2]
            t8a = top[:, hp * 16:hp * 16 + 8]; t8b = top[:, hp * 16 + 8:hp * 16 + 16]
            i8a = idxu[:, hp * 16:hp * 16 + 8]; i8b = idxu[:, hp * 16 + 8:hp * 16 + 16]
            return [
                lambda: fw.op(dve, lambda e: e.max(out=t8a, in_=sv), reads=[RSC], writes=[RTOPs[hp]]),
                lambda: fw.op(dve, lambda e: e.max_index(out=i8a, in_max=t8a, in_values=sv), reads=[RSC, RTOPs[hp]], writes=[RIDXs[hp]]),
                lambda: fw.op(dve, lambda e: e.match_replace(out=wk, in_to_replace=t8a, in_values=sv, imm_value=-1e30), reads=[RSC, RTOPs[hp]], writes=[rwk]),
                lambda: fw.op(dve, lambda e: e.max(out=t8b, in_=wk), reads=[rwk], writes=[RTOPs[hp]]),
                lambda: fw.op(dve, lambda e: e.max_index(out=i8b, in_max=t8b, in_values=wk), reads=[rwk, RTOPs[hp]], writes=[RIDXs[hp]]),
            ]

        for hp in range(0, 16, 2):
            ca, cb = topk_chain(hp), topk_chain(hp + 1)
            for ta, tb in zip(ca, cb):
                ta(); yield
                tb(); yield
        fw.op(dve, lambda e: e.tensor_copy(out=idxf, in_=idxu), reads=RIDXs, writes=[RIF])
        yield
        fw.op(dve, lambda e: e.tensor_scalar(out=idxf128, in0=idxf, scalar1=128.0, scalar2=None, op0=ALU.mult), reads=[RIF], writes=[RIF])
        yield

        def head_chain(hh):
            S = HS[hh % 2]
            cand_h, cidx_h, work_h, posu_h, posf_h = S["cand"], S["cidx"], S["work2"], S["posu"], S["posf"]
            rcd, rci, rwk2, rpos, rposf = S["RCD"], S["RCI"], S["RWK2"], S["RPOS"], S["RPOSF"]
            a0 = (2 * hh) * 16; b0 = (2 * hh + 1) * 16
            c8a = ctop[:, hh * 16:hh * 16 + 8]; c8b = ctop[:, hh * 16 + 8:hh * 16 + 16]
            ops = []
            for (aa0, na, nb, off) in RECTS:
                def mk(aa0=aa0, na=na, nb=nb, off=off):
                    cv = cand_h[:, off:off + na * nb].rearrange("p (a b) -> p a b", a=na, b=nb)
                    iv = cidx_h[:, off:off + na * nb].rearrange("p (a b) -> p a b", a=na, b=nb)
                    return [
                        lambda: fw.op(dve, lambda e: e.tensor_tensor(out=cv, in0=top[:, a0 + aa0:a0 + aa0 + na].unsqueeze(2).to_broadcast([128, na, nb]), in1=top[:, b0:b0 + nb].unsqueeze(1).to_broadcast([128, na, nb]), op=ALU.add), reads=[RTOPs[2 * hh], RTOPs[2 * hh + 1]], writes=[rcd]),
                        lambda: fw.op(dve, lambda e: e.tensor_tensor(out=iv, in0=idxf128[:, a0 + aa0:a0 + aa0 + na].unsqueeze(2).to_broadcast([128, na, nb]), in1=idxf[:, b0:b0 + nb].unsqueeze(1).to_broadcast([128, na, nb]), op=ALU.add), reads=[RIF], writes=[rci]),
                    ]
                ops += mk()
            ops += [
                lambda: fw.op(dve, lambda e: e.max(out=c8a, in_=cand_h), reads=[rcd], writes=[RCTs[hh]]),
                lambda: fw.op(dve, lambda e: e.match_replace(out=work_h, in_to_replace=c8a, in_values=cand_h, imm_value=-1e30), reads=[rcd, RCTs[hh]], writes=[rwk2]),
                lambda: fw.op(dve, lambda e: e.max(out=c8b, in_=work_h), reads=[rwk2], writes=[RCTs[hh]]),
                lambda: fw.op(dve, lambda e: e.max_index(out=posu_h[:, 0:8], in_max=c8a, in_values=cand_h), reads=[rcd, RCTs[hh]], writes=[rpos]),
                lambda: fw.op(dve, lambda e: e.max_index(out=posu_h[:, 8:16], in_max=c8b, in_values=work_h), reads=[rwk2, RCTs[hh]], writes=[rpos]),
                lambda: fw.op(dve, lambda e: e.tensor_copy(out=posf_h, in_=posu_h), reads=[rpos], writes=[rposf]),
            ]
            for k in range(16):
                def mk2(k=k):
                    def f():
                        jj = jc[0] % NJ; jc[0] += 1
                        fw.op(dve, lambda e: e.scalar_tensor_tensor(out=junkc[jj], in0=iota256[:, 0:NCAND], scalar=posf_h[:, k:k + 1], in1=cidx_h, op0=ALU.is_equal, op1=ALU.mult, accum_out=eidxf[:, hh * 16 + k:hh * 16 + k + 1]), reads=[RC, rposf, rci], writes=[RJ2[jj], REI])
                    return f
                ops.append(mk2())
            return ops

        for hh in range(0, 8, 2):
            ca, cb = head_chain(hh), head_chain(hh + 1)
            for ta, tb in zip(ca, cb):
                ta(); yield
                tb(); yield
        fw.op(dve, lambda e: e.tensor_scalar(out=eidxf, in0=eidxf, scalar1=16383.0, scalar2=0.0, op0=ALU.min, op1=ALU.max), reads=[REI], writes=[REI])
        yield
        fw.op(dve, lambda e: e.tensor_copy(out=eidx_r[slot], in_=eidxf), reads=[REI], writes=[REA[slot]])
        yield
        for hh in range(8):
            fw.op(dve, lambda e: e.tensor_scalar(out=negmax[:, hh:hh + 1], in0=ctop[:, hh * 16:hh * 16 + 1], scalar1=-1.0, scalar2=None, op0=ALU.mult), reads=[RCTs[hh]], writes=[RSM])
            yield
        for hh in range(8):
            fw.op(act, lambda e: e.activation(out=gt[:, hh * 16:(hh + 1) * 16], in_=ctop[:, hh * 16:(hh + 1) * 16], func=AF.Exp, bias=negmax[:, hh:hh + 1], accum_out=gs[:, hh:hh + 1]), reads=[RCTs[hh], RSM], writes=[RG, RSM])
        fw.op(dve, lambda e: e.reciprocal(out=rg, in_=gs), reads=[RSM], writes=[RSM])
        yield
        for hh in range(8):
            fw.op(dve, lambda e: e.tensor_scalar(out=gt[:, hh * 16:(hh + 1) * 16], in0=gt[:, hh * 16:(hh + 1) * 16], scalar1=rg[:, hh:hh + 1], scalar2=None, op0=ALU.mult), reads=[RG, RSM], writes=[RG])
            yield

    NG = 10
    uvb = [fw.alloc([2048], BF16) for _ in range(NG)]; RUV = _regs(NG, "uvb")
    ND = 4
    diag = [fw.alloc([128], BF16) for _ in range(ND)]; RDG = _regs(ND, "diag")
    acol = [fw.alloc([4], F32) for _ in range(ND)]; RAC = _regs(ND, "acol")
    hn_tok = [fw.alloc([1024], BF16) for _ in range(1)]; RHT = _regs(1, "hn_tok")
    NJB = 2
    junkb = [fw.alloc([1024], BF16) for _ in range(NJB)]; RJ1 = _regs(NJB, "junkb")
    acc = fw.alloc([1024], F32); RACC = Reg("acc")
    obuf = [fw.alloc([1024], F32) for _ in range(2)]; ROB = _regs(2, "obuf")
    st4 = fw.alloc([4], F32); RS4 = Reg("st4")
    gi = [0]

    def gather(blk):
        tk = slice(blk * 128, (blk + 1) * 128)
        slot = blk % NR
        ht = hn_tok[0]; rht = RHT[0]
        fw.op(dve, lambda e: e.scalar_tensor_tensor(out=ht, in0=hT[:, blk, :], scalar=rstd_all[:, blk:blk + 1], in1=fnw_rep, op0=ALU.mult, op1=ALU.mult), reads=[RH[blk], RRS, RC], writes=[rht])
        pa = (ps[0], ps[1]) if blk % 2 == 0 else (ps[2], ps[3])
        rpa = (PS[0], PS[1]) if blk % 2 == 0 else (PS[2], PS[3])
        for hk in range(128):
            b = gi[0] % NG; d = gi[0] % ND; jb = gi[0] % NJB; gi[0] += 1
            fw.dma(pool, lambda e: e.indirect_dma_start(out=uvb[b], out_offset=None, in_=uvs_d, in_offset=bass.IndirectOffsetOnAxis(ap=eidx_r[slot][:, hk:hk + 1], axis=0)), reads=[REA[slot]], writes=[RUV[b]])
            fw.op(dve, lambda e: e.scalar_tensor_tensor(out=junkb[jb], in0=uvb[b][:, 0:1024], scalar=1.0, in1=ht, op0=ALU.mult, op1=ALU.mult, accum_out=acol[d][:, 0:1]), reads=[RUV[b], rht], writes=[RJ1[jb], RAC[d]])
            fw.op(act, lambda e: e.activation(out=acol[d][:, 1:2], in_=acol[d][:, 0:1], func=AF.Gelu), reads=[RAC[d]], writes=[RAC[d]])
            fw.op(act, lambda e: e.activation(out=acol[d][:, 2:3], in_=acol[d][:, 1:2], func=AF.Copy, scale=g_r[slot][:, hk:hk + 1]), reads=[RAC[d], RGA[slot]], writes=[RAC[d]])
            fw.op(act, lambda e: e.activation(out=diag[d], in_=ident, func=AF.Copy, scale=acol[d][:, 2:3]), reads=[RCI, RAC[d]], writes=[RDG[d]])
            for half in range(2):
                fw.op(pe, lambda e: e.matmul(out=pa[half][:, 0:512], lhsT=diag[d], rhs=uvb[b][:, 1024 + half * 512:1024 + (half + 1) * 512], start=(hk == 0), stop=(hk == 127)), reads=[RDG[d], RUV[b]], writes=[rpa[half]])
            yield
        for half in range(2):
            fw.op(dve, lambda e: e.tensor_tensor(out=acc[:, half * 512:(half + 1) * 512], in0=pa[half][:, 0:512], in1=hT[:, blk, half * 512:(half + 1) * 512], op=ALU.add), reads=[rpa[half], RH[blk]], writes=[RACC])
        ob = obuf[blk % 2]; rob = ROB[blk % 2]
        rms_rstd(acc, 1024, st4, RS4, ob, rob, [RACC])
        fw.op(dve, lambda e: e.scalar_tensor_tensor(out=ob, in0=acc, scalar=st4[:, 2:3], in1=finw_rep, op0=ALU.mult, op1=ALU.mult), reads=[RACC, RS4, RC], writes=[rob])
        outs.append(fw.dma(sp, lambda e: e.dma_start(out=out_d[tk, :], in_=ob), reads=[rob]))

    RSTEPS = RSTEPS_CFG[0]
    if P4STOP[0] == 5:
        def load_h(blk, xs, rx):
            return hT[:, blk, :], [RH[blk]]
        RTMP = _regs(1, "tmpdst")
        norm_transpose(load_h, 1, lambda blk, kc: hnb[0][:, kc, :], fnw, RTMP)
        fw.barrier(); fw.finish(outs); return nc
    if P4STOP[0] == 6:
        xn = xnb[0]
        fw.op(act, lambda e: e.activation(out=xn, in_=hT[:, 0, :], func=AF.Copy, scale=rstd_all[:, 0:1]), reads=[RH[0], RRS], writes=[RXB[0]])
        fw.barrier(); fw.finish(outs); return nc
    if P4STOP[0] in (7, 8, 9):
        xn = xnb[0]
        fw.op(act, lambda e: e.activation(out=xn, in_=hT[:, 0, :], func=AF.Copy, scale=rstd_all[:, 0:1]), reads=[RH[0], RRS], writes=[RXB[0]])
        for kc in range(8):
            fw.op(pe, lambda e: e.transpose(out=pt[0][:, kc * 128:(kc + 1) * 128], in_=xn[:, kc * 128:(kc + 1) * 128], identity=ident), reads=[RXB[0], RCI], writes=[PT[0]])
        if P4STOP[0] == 8:
            for kc in range(0, 8, 2):
                fw.op(dve, lambda e: e.tensor_scalar(out=hnb[0][:, kc, :], in0=pt[0][:, kc * 128:(kc + 1) * 128], scalar1=fnw[:, kc:kc + 1], scalar2=None, op0=ALU.mult), reads=[PT[0], RC], writes=[RHB[0][kc]])
        if P4STOP[0] == 9:
            for kc in range(1, 8, 2):
                fw.op(act, lambda e: e.activation(out=hnb[0][:, kc, :], in_=pt[0][:, kc * 128:(kc + 1) * 128], func=AF.Copy, scale=fnw[:, kc:kc + 1]), reads=[PT[0], RC], writes=[RHB[0][kc]])
        fw.barrier(); fw.finish(outs); return nc
    route_pre(0)
    if P4STOP[0] == 2:
        fw.barrier(); fw.finish(outs); return nc
    for _ in route(0):
        pass
    if P4STOP[0] == 3:
        fw.barrier(); fw.finish(outs); return nc
    if P4STOP[0] == 4:
        for _ in gather(0):
            pass
        fw.barrier(); fw.finish(outs); return nc
    for blk in range(16):
        if blk + 1 < 16:
            route_pre(blk + 1)
        rr = route(blk + 1) if blk + 1 < 16 else None
        for _ in gather(blk):
            if rr is not None:
                for _i in range(RSTEPS):
                    try:
                        next(rr)
                    except StopIteration:
                        rr = None
                        break
        if rr is not None:
            for _ in rr:
                pass
    fw.finish(outs)
    return nc


_CACHE = {}


def _prep_shared(inp):
    f = np.float32
    w_in = np.asarray(inp["w_in"], f)[0]
    perm = np.array([m * 64 + (d + 32) % 64 for m in range(2) for d in range(64)])
    wA = []
    for h in range(4):
        q = w_in[:, h * 128:(h + 1) * 128]
        k = w_in[:, 512 + h * 128:512 + (h + 1) * 128]
        v = w_in[:, 1024 + h * 128:1024 + (h + 1) * 128]
        wA.append(np.concatenate([q, q[:, perm], k, k[:, perm], v], axis=1).reshape(8, 128, 640))
    wA = np.ascontiguousarray(np.stack(wA))
    bg = w_in[:, 1536:2048]; cg = w_in[:, 2048:2560]; xc = w_in[:, 2560:3072]
    ga = w_in[:, 3072:4096]; gb = w_in[:, 4096:5120]
    wC = np.ascontiguousarray(np.stack([np.concatenate([bg[:, c * 128:(c + 1) * 128], cg[:, c * 128:(c + 1) * 128], xc[:, c * 128:(c + 1) * 128]], axis=1).reshape(8, 128, 384) for c in range(4)]))
    wG = np.ascontiguousarray(np.stack([np.concatenate([ga[:, c * 128:(c + 1) * 128], gb[:, c * 128:(c + 1) * 128]], axis=1).reshape(8, 128, 256) for c in range(8)]))
    sh = dict(
        ident=np.eye(128, dtype=f),
        iota256=np.ascontiguousarray(np.broadcast_to(np.arange(256, dtype=f)[None, :], (128, 256))),
        anw=np.ascontiguousarray(np.asarray(inp["attn_norm_w"], f)[0].reshape(8, 128).T),
        fnw=np.ascontiguousarray(np.asarray(inp["ffn_norm_w"], f)[0].reshape(8, 128).T),
        fnw_rep=np.ascontiguousarray(np.broadcast_to(np.asarray(inp["ffn_norm_w"], f)[0][None, :], (128, 1024))),
        finw_rep=np.ascontiguousarray(np.broadcast_to(np.asarray(inp["final_norm_w"], f)[None, :], (128, 1024))),
        subln_rep=np.ascontiguousarray(np.broadcast_to(np.asarray(inp["subln_w"], f)[0][None, :], (128, 128))),
        subcol=np.ascontiguousarray(np.asarray(inp["subln_w"], f)[0].reshape(128, 1)),
        lam_in=np.ascontiguousarray(np.broadcast_to(np.stack([np.asarray(inp[k], f)[0] for k in ("lambda_q1", "lambda_k1", "lambda_q2", "lambda_k2")])[None], (128, 4, 64))),
        convw=np.ascontiguousarray(np.asarray(inp["conv_w"], f)[0].reshape(3, 4, 128).transpose(2, 1, 0)),
        wA=wA, wC=wC, wG=wG,
        wpa=np.ascontiguousarray(np.asarray(inp["w_proj_attn"], f)[0].reshape(4, 128, 1024)),
        wpb=np.ascontiguousarray(np.asarray(inp["w_proj_conv"], f)[0].reshape(4, 128, 1024)),
        wo=np.ascontiguousarray(np.asarray(inp["w_out"], f)[0].reshape(8, 128, 1024)),
        wq=np.ascontiguousarray(np.asarray(inp["w_query"], f)[0].reshape(8, 128, 2048)),
        skT=np.ascontiguousarray(np.asarray(inp["sub_keys"], f)[0].reshape(16, 128, 128).transpose(2, 0, 1)),
        expert_u=np.ascontiguousarray(np.asarray(inp["expert_u"], f)[0]),
        expert_v=np.ascontiguousarray(np.asarray(inp["expert_v"], f)[0]),
    )
    return sh


def _rope_tables():
    inv_freq = (1.0 / (10000.0 ** (np.arange(0, 64, 2, dtype=np.float32) / np.float32(64)))).astype(np.float32)
    pos = np.arange(SEQ, dtype=np.float32)
    ang = (pos[:, None] * inv_freq[None, :]).astype(np.float32)
    d = np.arange(128) % 64
    cos = np.cos(ang)[:, d % 32].T.astype(np.float32)
    sin = np.sin(ang)[:, d % 32].T.astype(np.float32)
    sign = np.where(d < 32, -1.0, 1.0).astype(np.float32)[:, None]
    return cos, (sin * sign).astype(np.float32)


def _core_inputs(inp, sh, cos, sin, c):
    b, half = c // 2, c % 2
    x = np.asarray(inp["x"], np.float32)
    own = slice(half * T_OWN, (half + 1) * T_OWN)
    oth = slice((1 - half) * T_OWN, (2 - half) * T_OWN)
    m = dict(sh)
    m["xs"] = np.ascontiguousarray(np.concatenate([x[b, own], x[b, oth]], axis=0))
    m["cosT"] = np.ascontiguousarray(np.concatenate([cos[:, own], cos[:, oth]], axis=1))
    m["sinT"] = np.ascontiguousarray(np.concatenate([sin[:, own], sin[:, oth]], axis=1))
    fl = np.zeros((128, 2), np.float32)
    fl[:, 0] = 1.0 if half == 1 else 0.0
    fl[:, 1] = 1.0 if half == 0 else 0.0
    m["flags"] = fl
    return m


def kernel(**inputs):
    if "nc" not in _CACHE:
        _CACHE["nc"] = build_program(dbg=False)
    nc = _CACHE["nc"]
    sh = _prep_shared(inputs)
    cos, sin = _rope_tables()
    in_maps = [_core_inputs(inputs, sh, cos, sin, c) for c in range(8)]
    res = run_bass_kernel_spmd(nc, in_maps, core_ids=list(range(8)))
    out = np.empty((NB, SEQ, D_MODEL), np.float32)
    for c in range(8):
        b, half = c // 2, c % 2
        out[b, half * T_OWN:(half + 1) * T_OWN] = np.asarray(res.results[c]["out"], np.float32)
    return out
```

```python
import numpy as np
import ml_dtypes
import concourse.bass as bass
import concourse.mybir as mybir
from concourse.bass_utils import run_bass_kernel_spmd

F32 = mybir.dt.float32
BF16 = mybir.dt.bfloat16
I32 = mybir.dt.int32
U32 = mybir.dt.uint32
U8 = mybir.dt.uint8
AF = mybir.ActivationFunctionType
ALU = mybir.AluOpType
AX = mybir.AxisListType
DTSIZE = {F32: 4, BF16: 2, I32: 4, U32: 4, U8: 1}


STRICT = True


class Reg:
    __slots__ = ("name", "last_w", "readers", "disjoint")

    def __init__(self, name="", disjoint=False):
        self.name = name
        self.last_w = None
        self.readers = []
        self.disjoint = disjoint


class Eng:
    def __init__(self, fw, name, h, ndma=0):
        self.fw = fw
        self.name = name
        self.h = h
        self.sem = fw.nc.alloc_semaphore("s_" + name)
        self.count = 0
        self.waited = {}
        self.dsems = [fw.nc.alloc_semaphore("d_%s%d" % (name, i)) for i in range(ndma)]
        self.dcount = [0] * ndma
        self.drr = 0

    def wait(self, ev):
        if ev is None:
            return
        _, sem, val = ev
        k = id(sem)
        if self.waited.get(k, 0) >= val:
            return
        self.h.wait_ge(sem, val)
        self.waited[k] = val


class FW:
    def __init__(self, nc, arena_bytes=200 * 1024):
        self.nc = nc
        self.pe = Eng(self, "pe", nc.tensor)
        self.act = Eng(self, "act", nc.scalar)
        self.dve = Eng(self, "dve", nc.vector)
        self.pool = Eng(self, "pool", nc.gpsimd, ndma=12)
        self.sp = Eng(self, "sp", nc.sync, ndma=8)
        self.engs = [self.pe, self.act, self.dve, self.pool, self.sp]
        self.arena = nc.alloc_sbuf_tensor("arena", [128, arena_bytes], U8)
        self.arena_bytes = arena_bytes
        self.top = 0
        self.ps_tensors = []

    def mark(self):
        return self.top

    def release(self, m):
        self.top = m

    def alloc(self, shape, dtype, parts=128):
        n = int(np.prod(shape)) * DTSIZE[dtype]
        n_al = (n + 63) // 64 * 64
        off = self.top
        assert off + n_al <= self.arena_bytes, ("SBUF arena overflow", off, n_al)
        self.top = off + n_al
        ap = self.arena[0:parts, off:off + n].bitcast(dtype)
        if len(shape) > 1:
            names = " ".join("d%d" % i for i in range(len(shape)))
            kw = {"d%d" % i: int(s) for i, s in enumerate(shape)}
            ap = ap.rearrange("p (%s) -> p %s" % (names, names), **kw)
        return ap

    def alloc_at(self, off, shape, dtype, parts=128):
        n = int(np.prod(shape)) * DTSIZE[dtype]
        ap = self.arena[0:parts, off:off + n].bitcast(dtype)
        if len(shape) > 1:
            names = " ".join("d%d" % i for i in range(len(shape)))
            kw = {"d%d" % i: int(s) for i, s in enumerate(shape)}
            ap = ap.rearrange("p (%s) -> p %s" % (names, names), **kw)
        return ap

    def _deps(self, eng, reads, writes, is_dma=False):
        inorder = (not is_dma) and (eng.name == "pe" or (not STRICT and eng.name in ("act", "dve")))
        deps = []
        for r in reads:
            if r.last_w is not None:
                deps.append(r.last_w)
        for w in writes:
            if w.last_w is not None:
                same = (not is_dma) and w.last_w[0] == eng.name
                if not (same and (inorder or w.disjoint)):
                    deps.append(w.last_w)
            for rd in w.readers:
                if inorder and rd[0] == eng.name:
                    continue
                deps.append(rd)
        return deps

    def op(self, eng, fn, reads=(), writes=()):
        deps = self._deps(eng, reads, writes)
        for d in deps:
            if eng.name == "pe" and d[0] == "pe":
                continue
            eng.wait(d)
        ins = fn(eng.h)
        eng.count += 1
        ins.then_inc(eng.sem, 1)
        ev = (eng.name, eng.sem, eng.count)
        self._record(ev, reads, writes)
        return ev

    def _record(self, ev, reads, writes):
        for r in reads:
            r.readers = [x for x in r.readers if x[1] is not ev[1]] + [ev]
        for w in writes:
            w.last_w = ev
            w.readers = []

    def dma(self, eng, fn, reads=(), writes=()):
        deps = self._deps(eng, reads, writes, is_dma=True)
        for d in deps:
            eng.wait(d)
        i = eng.drr
        eng.drr = (i + 1) % len(eng.dsems)
        sem = eng.dsems[i]
        if eng.dcount[i] > 0:
            eng.wait(("dma", sem, eng.dcount[i]))
        ins = fn(eng.h)
        eng.dcount[i] += 16
        ins.then_inc(sem, 16)
        ev = ("dma", sem, eng.dcount[i])
        self._record(ev, reads, writes)
        return ev

    def barrier(self):
        evs = []
        for e in self.engs:
            if e.count:
                evs.append((e.name, e.sem, e.count))
            for i, s in enumerate(e.dsems):
                if e.dcount[i]:
                    evs.append(("dma", s, e.dcount[i]))
        for e in self.engs:
            for ev in evs:
                e.wait(ev)

    def finish(self, out_evs):
        for ev in out_evs:
            self.sp.wait(ev)
            self.pool.wait(ev)

D_MODEL = 1024
SEQ = 4096
NB = 4
T_OWN = 2048
EPS = 1e-6
LAMBDA_INIT = 0.8 - 0.6 * 1.0


def _regs(n, name):
    return [Reg("%s%d" % (name, i)) for i in range(n)]


RSTEPS_CFG = [3]
P4STOP = [0]


def build_program(dbg=False):
    nc = bass.Bass("TRN2", target_bir_lowering=False)

    def DI(name, shape, dt=F32):
        return nc.dram_tensor(name, list(shape), dt, kind="ExternalInput").ap()

    xs_d = DI("xs", [4096, 1024])
    cos_d = DI("cosT", [128, 4096])
    sin_d = DI("sinT", [128, 4096])
    flags_d = DI("flags", [128, 2])
    ident_d = DI("ident", [128, 128])
    iota_d = DI("iota256", [128, 256])
    anw_d = DI("anw", [128, 8])
    fnw_d = DI("fnw", [128, 8])
    fnw_rep_d = DI("fnw_rep", [128, 1024])
    finw_rep_d = DI("finw_rep", [128, 1024])
    subln_d = DI("subln_rep", [128, 128])
    subcol_d = DI("subcol", [128, 1])
    lam_d = DI("lam_in", [128, 4, 64])
    convw_d = DI("convw", [128, 4, 3])
    wA_d = DI("wA", [4, 8, 128, 640])
    wC_d = DI("wC", [4, 8, 128, 384])
    wG_d = DI("wG", [8, 8, 128, 256])
    wpa_d = DI("wpa", [4, 128, 1024])
    wpb_d = DI("wpb", [4, 128, 1024])
    wo_d = DI("wo", [8, 128, 1024])
    wq_d = DI("wq", [8, 128, 2048])
    sk_d = DI("skT", [128, 16, 128])
    eu_d = DI("expert_u", [16384, 1024])
    ev_d = DI("expert_v", [16384, 1024])
    uvs_d = nc.dram_tensor("uvs", [16384, 2048], BF16, kind="Internal").ap()
    out_d = nc.dram_tensor("out", [T_OWN, 1024], F32, kind="ExternalOutput").ap()
    dbg_d = {}
    if dbg:
        dbg_d["attnT"] = nc.dram_tensor("dbg_attnT", [128, 4, 2048], F32, kind="ExternalOutput").ap()
        dbg_d["h"] = nc.dram_tensor("dbg_h", [128, 16, 1024], F32, kind="ExternalOutput").ap()
        dbg_d["eidx"] = nc.dram_tensor("dbg_eidx", [128, 128], I32, kind="ExternalOutput").ap()
        dbg_d["g"] = nc.dram_tensor("dbg_g", [128, 128], F32, kind="ExternalOutput").ap()
        dbg_d["xnT"] = nc.dram_tensor("dbg_xnT", [128, 8, 2048], F32, kind="ExternalOutput").ap()
        dbg_d["uvs"] = nc.dram_tensor("dbg_uvs", [3, 128, 2048], BF16, kind="ExternalOutput").ap()

    fw = FW(nc, arena_bytes=207 * 1024)
    sp, pool, act, dve, pe = fw.sp, fw.pool, fw.act, fw.dve, fw.pe
    ps = [nc.alloc_psum_tensor("ps%d" % i, [128, 512], F32) for i in range(6)]
    pt = [nc.alloc_psum_tensor("pt%d" % i, [128, 1024], BF16) for i in range(2)]
    PS = _regs(6, "ps")
    PT = _regs(2, "pt")
    outs = []

    def dma_in(dst, src, reg, eng=None):
        return fw.dma(eng or sp, lambda e: e.dma_start(out=dst, in_=src), writes=[reg])

    def dbg_dump(name, src_ap, reg, shape):
        if not dbg:
            return
        dst = dbg_d[name]
        outs.append(fw.dma(sp, lambda e: e.dma_start(out=dst, in_=src_ap), reads=[reg]))

    ident_f = fw.alloc([128], F32); ident = fw.alloc([128], BF16)
    anw = fw.alloc([8], F32); fnw = fw.alloc([8], F32)
    subln = fw.alloc([128], F32); flags = fw.alloc([2], F32)
    lam_in = fw.alloc([4, 64], F32); lam_t = fw.alloc([8], F32); lam_j = fw.alloc([64], F32)
    convw = fw.alloc([4, 3], F32)
    RC = Reg("consts")
    for dst, src in ((ident_f, ident_d), (anw, anw_d), (fnw, fnw_d), (subln, subln_d), (flags, flags_d),
                     (lam_in, lam_d), (convw, convw_d)):
        dma_in(dst, src, RC)
    RCI = Reg("ident")
    fw.barrier()
    fw.op(dve, lambda e: e.tensor_copy(out=ident, in_=ident_f), reads=[RC], writes=[RCI])
    fw.op(dve, lambda e: e.tensor_scalar(out=subln, in0=subln, scalar1=1.0 - LAMBDA_INIT, scalar2=None, op0=ALU.mult), reads=[RC], writes=[RC])
    fw.op(dve, lambda e: e.scalar_tensor_tensor(out=lam_j, in0=lam_in[:, 0, :], scalar=1.0, in1=lam_in[:, 1, :], op0=ALU.mult, op1=ALU.mult, accum_out=lam_t[:, 0:1]), reads=[RC], writes=[RC])
    fw.op(dve, lambda e: e.scalar_tensor_tensor(out=lam_j, in0=lam_in[:, 2, :], scalar=1.0, in1=lam_in[:, 3, :], op0=ALU.mult, op1=ALU.mult, accum_out=lam_t[:, 1:2]), reads=[RC], writes=[RC])
    fw.op(act, lambda e: e.activation(out=lam_t[:, 2:4], in_=lam_t[:, 0:2], func=AF.Exp), reads=[RC], writes=[RC])
    fw.op(dve, lambda e: e.tensor_tensor(out=lam_t[:, 4:5], in0=lam_t[:, 3:4], in1=lam_t[:, 2:3], op=ALU.subtract), reads=[RC], writes=[RC])
    fw.op(dve, lambda e: e.tensor_scalar(out=lam_t[:, 4:5], in0=lam_t[:, 4:5], scalar1=-LAMBDA_INIT, scalar2=None, op0=ALU.add), reads=[RC], writes=[RC])
    neglam = lam_t[:, 4:5]

    off_own = fw.top
    xnT_own = fw.alloc([8, 2048], BF16)
    attnT = fw.alloc([4, 2048], BF16)
    ucT = fw.alloc([4, 2048], BF16)
    off_oth = fw.top
    xnT_oth = fw.alloc([8, 2048], BF16)
    m_pers = fw.mark()
    RXN = [Reg("xnT%d" % i, disjoint=True) for i in range(32)]
    stat = [fw.alloc([4], F32) for _ in range(4)]
    RST = _regs(4, "stat")
    m_tmp = fw.mark()

    def rms_rstd(src_ap, n, st, rst, junk, rjunk, src_regs):
        fw.op(act, lambda e: e.activation(out=junk, in_=src_ap, func=AF.Square, accum_out=st[:, 0:1]), reads=src_regs, writes=[rjunk, rst])
        fw.op(dve, lambda e: e.tensor_scalar(out=st[:, 1:2], in0=st[:, 0:1], scalar1=1.0 / n, scalar2=EPS, op0=ALU.mult, op1=ALU.add), reads=[rst], writes=[rst])
        fw.op(act, lambda e: e.activation(out=st[:, 3:4], in_=st[:, 1:2], func=AF.Ln), reads=[rst], writes=[rst])
        fw.op(act, lambda e: e.activation(out=st[:, 2:3], in_=st[:, 3:4], func=AF.Exp, scale=-0.5), reads=[rst], writes=[rst])

    def norm_transpose(load_fn, nblk, dst_fn, wcol, regs_dst, post_fn=None):
        xbuf = [fw.alloc([1024], F32) for _ in range(4)]; RX = _regs(4, "xbuf")
        xnb = [fw.alloc([1024], BF16) for _ in range(2)]; RXB = _regs(2, "xnb")
        junk = fw.alloc([1024], BF16); RJ = Reg("junk")
        srcs = {}

        def stage_a(blk):
            xs = xbuf[blk % 4]; rx = RX[blk % 4]
            src_ap, src_regs = load_fn(blk, xs, rx)
            srcs[blk] = (src_ap, src_regs)
            st = stat[blk % 4]; rst = RST[blk % 4]
            fw.op(act, lambda e: e.activation(out=junk, in_=src_ap, func=AF.Square, accum_out=st[:, 0:1]), reads=src_regs, writes=[RJ, rst])
            fw.op(dve, lambda e: e.tensor_scalar(out=st[:, 1:2], in0=st[:, 0:1], scalar1=1.0 / 1024, scalar2=EPS, op0=ALU.mult, op1=ALU.add), reads=[rst], writes=[rst])

        def stage_b(blk):
            src_ap, src_regs = srcs.pop(blk)
            st = stat[blk % 4]; rst = RST[blk % 4]
            fw.op(act, lambda e: e.activation(out=st[:, 3:4], in_=st[:, 1:2], func=AF.Ln), reads=[rst], writes=[rst])
            fw.op(act, lambda e: e.activation(out=st[:, 2:3], in_=st[:, 3:4], func=AF.Exp, scale=-0.5), reads=[rst], writes=[rst])
            if post_fn is not None:
                post_fn(blk, st, rst)
            xn = xnb[blk % 2]; rxn = RXB[blk % 2]
            fw.op(act, lambda e: e.activation(out=xn, in_=src_ap, func=AF.Copy, scale=st[:, 2:3]), reads=src_regs + [rst], writes=[rxn])
            p = pt[blk % 2]; rp = PT[blk % 2]
            for kc in range(8):
                fw.op(pe, lambda e: e.transpose(out=p[:, kc * 128:(kc + 1) * 128], in_=xn[:, kc * 128:(kc + 1) * 128], identity=ident), reads=[rxn, RCI], writes=[rp])
            for kc in range(8):
                fw.op(dve, lambda e: e.tensor_scalar(out=dst_fn(blk, kc), in0=p[:, kc * 128:(kc + 1) * 128], scalar1=wcol[:, kc:kc + 1], scalar2=None, op0=ALU.mult), reads=[rp, RC], writes=[regs_dst[blk]])

        stage_a(0)
        for blk in range(nblk):
            if blk + 1 < nblk:
                stage_a(blk + 1)
            stage_b(blk)

    def load_x(blk, xs, rx):
        dma_in(xs, xs_d[blk * 128:(blk + 1) * 128, :], rx)
        return xs, [rx]

    def xn_dst(blk, kc):
        if blk < 16:
            return xnT_own[:, kc, blk * 128:(blk + 1) * 128]
        return xnT_oth[:, kc, (blk - 16) * 128:(blk - 15) * 128]

    norm_transpose(load_x, 32, xn_dst, anw, RXN)
    fw.release(m_tmp)
    fw.barrier()

    def xt_tile(t, kc):
        if t < 4:
            return xnT_own[:, kc, t * 512:(t + 1) * 512]
        return xnT_oth[:, kc, (t - 4) * 512:(t - 3) * 512]

    def xt_regs(t):
        return RXN[t * 4:(t + 1) * 4]

    cosT = fw.alloc([4096], F32); sinT = fw.alloc([4096], F32); RTAB = Reg("tab")
    dma_in(cosT, cos_d, RTAB); dma_in(sinT, sin_d, RTAB)
    subcol = fw.alloc([1], F32)
    dma_in(subcol, subcol_d, RC)
    ones_bf = fw.alloc([128], BF16); RONE = Reg("ones")
    fw.barrier()
    fw.op(dve, lambda e: e.tensor_scalar(out=subcol, in0=subcol, scalar1=1.0 - LAMBDA_INIT, scalar2=None, op0=ALU.mult), reads=[RC], writes=[RC])
    fw.op(pool, lambda e: e.memset(ones_bf, 1.0), writes=[RONE])
    wab = [fw.alloc([8, 640], BF16) for _ in range(2)]; RWAb = [_regs(8, "wa%d" % i) for i in range(2)]

    def load_wa(hd):
        for kc in range(8):
            fw.dma(pool, lambda e: e.dma_start(out=wab[hd % 2][:, kc, :], in_=wA_d[hd, kc]), writes=[RWAb[hd % 2][kc]])
    kT = fw.alloc([4096], BF16); RK = Reg("kT")
    qT = fw.alloc([2048], BF16); RQ = Reg("qT")
    vx = fw.alloc([4096], BF16); RV = Reg("vx")
    t1b = [fw.alloc([512], F32) for _ in range(2)]; RT1 = _regs(2, "t1")
    t2b = [fw.alloc([512], F32) for _ in range(2)]; RT2 = _regs(2, "t2")
    pTb = [fw.alloc([1024], BF16) for _ in range(3)]; RPT = _regs(3, "pT")
    accp = fw.alloc([1024], F32); RACP = Reg("accp")
    rz = fw.alloc([1024], F32); RRZ = Reg("rz")
    RAT = Reg("attnT")
    ropei = [0]
    cvt = fw.alloc([2, 2048], BF16)
    eu_v = eu_d.rearrange("(c p j) d -> c p j d", p=128, j=2)
    ev_v = ev_d.rearrange("(c p j) d -> c p j d", p=128, j=2)
    uvs_v = uvs_d.rearrange("(c p j) d -> c p j d", p=128, j=2)
    RCVu = Reg("cvtu"); RCVv = Reg("cvtv")

    def convert_chunk(c):
        fw.dma(pool, lambda e: e.dma_start(out=cvt[:, :, 0:1024], in_=eu_v[c]), writes=[RCVu])
        fw.dma(pool, lambda e: e.dma_start(out=cvt[:, :, 1024:2048], in_=ev_v[c]), writes=[RCVv])
        fw.dma(sp, lambda e: e.dma_start(out=uvs_v[c], in_=cvt), reads=[RCVu, RCVv])

    def rope(bank_a, ra, bank_b, rb, t, dst, rdst):
        i = ropei[0] % 2; ropei[0] += 1
        fw.op(dve, lambda e: e.tensor_tensor(out=t1b[i], in0=bank_a[:, 0:512], in1=cosT[:, t * 512:(t + 1) * 512], op=ALU.mult), reads=[ra, RTAB], writes=[RT1[i]])
        fw.op(dve, lambda e: e.tensor_tensor(out=t2b[i], in0=bank_b[:, 0:512], in1=sinT[:, t * 512:(t + 1) * 512], op=ALU.mult), reads=[rb, RTAB], writes=[RT2[i]])
        fw.op(pool, lambda e: e.tensor_tensor(out=dst, in0=t1b[i], in1=t2b[i], op=ALU.add), reads=[RT1[i], RT2[i]], writes=[rdst])

    cvi = [0]
    load_wa(0)
    for h in range(4):
        wa = wab[h % 2]; RWA = RWAb[h % 2]
        if h + 1 < 4:
            load_wa(h + 1)
        for t in range(8):
            groups = [(256, 0), (384, 1)] + ([(0, 2), (128, 3)] if t < 4 else [])
            for col0, bnk in groups:
                for kc in range(8):
                    fw.op(pe, lambda e: e.matmul(out=ps[bnk][:, 0:512], lhsT=wa[:, kc, col0:col0 + 128], rhs=xt_tile(t, kc), start=(kc == 0), stop=(kc == 7)), reads=[RWA[kc]] + xt_regs(t), writes=[PS[bnk]])
            rope(ps[0], PS[0], ps[1], PS[1], t, kT[:, t * 512:(t + 1) * 512], RK)
            if t < 4:
                rope(ps[2], PS[2], ps[3], PS[3], t, qT[:, t * 512:(t + 1) * 512], RQ)
            for j in range(4):
                for kc in range(8):
                    fw.op(pe, lambda e: e.matmul(out=ps[4][:, j * 128:(j + 1) * 128], lhsT=xt_tile(t, kc)[:, j * 128:(j + 1) * 128], rhs=wa[:, kc, 512:640], start=(kc == 0), stop=(kc == 7)), reads=[RWA[kc]] + xt_regs(t), writes=[PS[4]])
            fw.op(act, lambda e: e.activation(out=vx[:, t * 512:(t + 1) * 512], in_=ps[4][:, 0:512], func=AF.Copy), reads=[PS[4]], writes=[RV])
        for qt in range(4):
            q0 = qt * 512
            for _ in range(4):
                convert_chunk(cvi[0]); cvi[0] += 1

            def QK(kb):
                for m in range(2):
                    bnk = (kb % 2) * 2 + m
                    fw.op(pe, lambda e: e.matmul(out=ps[bnk][:, 0:512], lhsT=kT[m * 64:(m + 1) * 64, kb * 128:(kb + 1) * 128], rhs=qT[m * 64:(m + 1) * 64, q0:q0 + 512], start=True, stop=True), reads=[RK, RQ], writes=[PS[bnk]])

            def EXP(kb):
                for m in range(2):
                    bnk = (kb % 2) * 2 + m
                    fw.op(act, lambda e: e.activation(out=pTb[kb % 3][:, m * 512:(m + 1) * 512], in_=ps[bnk][:, 0:512], func=AF.Exp, scale=0.125), reads=[PS[bnk]], writes=[RPT[kb % 3]])

            def PV(kb):
                for m in range(2):
                    fw.op(pe, lambda e: e.matmul(out=ps[4 + m][:, 0:512], lhsT=vx[:, kb * 128:(kb + 1) * 128], rhs=pTb[kb % 3][:, m * 512:(m + 1) * 512], start=(kb == 0), stop=(kb == 31)), reads=[RPT[kb % 3], RV], writes=[PS[4 + m]])
                if kb == 0:
                    fw.op(dve, lambda e: e.tensor_copy(out=accp, in_=pTb[kb % 3]), reads=[RPT[kb % 3]], writes=[RACP])
                else:
                    fw.op(dve, lambda e: e.tensor_tensor(out=accp, in0=accp, in1=pTb[kb % 3], op=ALU.add), reads=[RPT[kb % 3], RACP], writes=[RACP])

            for kb in range(33):
                if kb < 32:
                    QK(kb); EXP(kb)
                if kb >= 1:
                    PV(kb - 1)
            hi, lo = pTb[0], pTb[1]
            fw.op(dve, lambda e: e.tensor_copy(out=hi, in_=accp), reads=[RACP], writes=[RPT[0]])
            fw.op(dve, lambda e: e.tensor_tensor(out=lo, in0=accp, in1=hi, op=ALU.subtract), reads=[RACP, RPT[0]], writes=[RPT[1]])
            for m in range(2):
                fw.op(pe, lambda e: e.matmul(out=ps[m][:, 0:512], lhsT=ones_bf, rhs=hi[:, m * 512:(m + 1) * 512], start=True, stop=False), reads=[RONE, RPT[0]], writes=[PS[m]])
                fw.op(pe, lambda e: e.matmul(out=ps[m][:, 0:512], lhsT=ones_bf, rhs=lo[:, m * 512:(m + 1) * 512], start=False, stop=True), reads=[RONE, RPT[1]], writes=[PS[m]])
                fw.op(dve, lambda e: e.reciprocal(out=rz[:, m * 512:(m + 1) * 512], in_=ps[m][:, 0:512]), reads=[PS[m]], writes=[RRZ])
            ta, tb2, tc, td = t1b[0], t2b[0], t1b[1], t2b[1]
            rta, rtb, rtc, rtd = RT1[0], RT2[0], RT1[1], RT2[1]
            fw.op(dve, lambda e: e.tensor_tensor(out=ta, in0=ps[4][:, 0:512], in1=rz[:, 0:512], op=ALU.mult), reads=[PS[4], RRZ], writes=[rta])
            fw.op(dve, lambda e: e.tensor_tensor(out=tb2, in0=ps[5][:, 0:512], in1=rz[:, 512:1024], op=ALU.mult), reads=[PS[5], RRZ], writes=[rtb])
            fw.op(dve, lambda e: e.scalar_tensor_tensor(out=tc, in0=tb2, scalar=neglam, in1=ta, op0=ALU.mult, op1=ALU.add), reads=[rtb, rta, RC], writes=[rtc])
            sq = pTb[2][:, 0:512]
            fw.op(act, lambda e: e.activation(out=sq, in_=tc, func=AF.Square), reads=[rtc], writes=[RPT[2]])
            fw.op(pe, lambda e: e.matmul(out=ps[2][:, 0:512], lhsT=ones_bf, rhs=sq, start=True, stop=True), reads=[RONE, RPT[2]], writes=[PS[2]])
            fw.op(dve, lambda e: e.tensor_scalar(out=td, in0=ps[2][:, 0:512], scalar1=1.0 / 128, scalar2=EPS, op0=ALU.mult, op1=ALU.add), reads=[PS[2]], writes=[rtd])
            fw.op(act, lambda e: e.activation(out=td, in_=td, func=AF.Ln), reads=[rtd], writes=[rtd])
            fw.op(act, lambda e: e.activation(out=td, in_=td, func=AF.Exp, scale=-0.5), reads=[rtd], writes=[rtd])
            fw.op(dve, lambda e: e.scalar_tensor_tensor(out=attnT[:, h, q0:q0 + 512], in0=tc, scalar=subcol[:, 0:1], in1=td, op0=ALU.mult, op1=ALU.mult), reads=[rtc, rtd, RC], writes=[RAT])
    fw.barrier()
    if dbg:
        stg = fw.alloc([2048], F32); RSTG = Reg("stg")
        for hh in range(4):
            fw.op(dve, lambda e: e.tensor_copy(out=stg, in_=attnT[:, hh, :]), reads=[RAT], writes=[RSTG])
            outs.append(fw.dma(sp, lambda e: e.dma_start(out=dbg_d["attnT"][:, hh, :], in_=stg), reads=[RSTG]))
        for kc in range(8):
            fw.op(dve, lambda e: e.tensor_copy(out=stg, in_=xnT_own[:, kc, :]), reads=RXN[0:16], writes=[RSTG])
            outs.append(fw.dma(sp, lambda e: e.dma_start(out=dbg_d["xnT"][:, kc, :], in_=stg), reads=[RSTG]))
        fw.barrier()
        stb = fw.alloc([2048], BF16); RSTB = Reg("stb")
        for i, r0 in enumerate((0, 640, 16256)):
            fw.dma(sp, lambda e: e.dma_start(out=stb, in_=uvs_d[r0:r0 + 128, :]), writes=[RSTB])
            outs.append(fw.dma(sp, lambda e: e.dma_start(out=dbg_d["uvs"][i], in_=stb), reads=[RSTB]))
        fw.barrier()
    fw.release(m_tmp)

    wcb = [fw.alloc([8, 384], BF16) for _ in range(2)]; RWCb = [_regs(8, "wc%d" % i) for i in range(2)]

    def load_wc(cc):
        for kc in range(8):
            fw.dma(pool, lambda e: e.dma_start(out=wcb[cc % 2][:, kc, :], in_=wC_d[cc, kc]), writes=[RWCb[cc % 2][kc]])

    zT = fw.alloc([2050], F32); RZ = Reg("zT")
    bgs = fw.alloc([2048], F32); RBG = Reg("bgs")
    tmpc = [fw.alloc([512], F32) for _ in range(2)]; RTC = _regs(2, "tmpc")
    c1 = fw.alloc([2048], F32); RC1 = Reg("c1"); c2 = fw.alloc([2048], F32); RC2 = Reg("c2")
    hs = fw.alloc([16], F32); RHS = Reg("hs")
    RUC = Reg("ucT")
    load_wc(0)
    for cc in range(4):
        wc = wcb[cc % 2]; RWC = RWCb[cc % 2]
        if cc + 1 < 4:
            load_wc(cc + 1)
        for t in range(4):
            for gi, col0 in enumerate((128, 256, 0)):
                for kc in range(8):
                    fw.op(pe, lambda e: e.matmul(out=ps[gi][:, 0:512], lhsT=wc[:, kc, col0:col0 + 128], rhs=xt_tile(t, kc), start=(kc == 0), stop=(kc == 7)), reads=[RWC[kc]] + xt_regs(t), writes=[PS[gi]])
            fw.op(act, lambda e: e.activation(out=tmpc[t % 2], in_=ps[0][:, 0:512], func=AF.Copy), reads=[PS[0]], writes=[RTC[t % 2]])
            fw.op(dve, lambda e: e.tensor_tensor(out=zT[:, 1 + t * 512:1 + (t + 1) * 512], in0=ps[1][:, 0:512], in1=tmpc[t % 2], op=ALU.mult), reads=[PS[1], RTC[t % 2]], writes=[RZ])
            fw.op(act, lambda e: e.activation(out=bgs[:, t * 512:(t + 1) * 512], in_=ps[2][:, 0:512], func=AF.Copy), reads=[PS[2]], writes=[RBG])
        for gi, (col0, tok) in enumerate(((128, 0), (256, 0), (128, 2044), (256, 2044))):
            for kc in range(8):
                fw.op(pe, lambda e: e.matmul(out=ps[3][:, gi * 4:gi * 4 + 4], lhsT=wc[:, kc, col0:col0 + 128], rhs=xnT_oth[:, kc, tok:tok + 4], start=(kc == 0), stop=(kc == 7)), reads=[RWC[kc]] + RXN[16:32], writes=[PS[3]])
        fw.op(dve, lambda e: e.tensor_copy(out=hs, in_=ps[3][:, 0:16]), reads=[PS[3]], writes=[RHS])
        fw.op(dve, lambda e: e.scalar_tensor_tensor(out=zT[:, 2049:2050], in0=hs[:, 0:1], scalar=flags[:, 1:2], in1=hs[:, 4:5], op0=ALU.mult, op1=ALU.mult), reads=[RHS, RC], writes=[RZ])
        fw.op(dve, lambda e: e.scalar_tensor_tensor(out=zT[:, 0:1], in0=hs[:, 11:12], scalar=flags[:, 0:1], in1=hs[:, 15:16], op0=ALU.mult, op1=ALU.mult), reads=[RHS, RC], writes=[RZ])
        fw.op(dve, lambda e: e.tensor_scalar(out=c1, in0=zT[:, 0:2048], scalar1=convw[:, cc, 0:1], scalar2=None, op0=ALU.mult), reads=[RZ, RC], writes=[RC1])
        fw.op(dve, lambda e: e.scalar_tensor_tensor(out=c2, in0=zT[:, 1:2049], scalar=convw[:, cc, 1:2], in1=c1, op0=ALU.mult, op1=ALU.add), reads=[RZ, RC, RC1], writes=[RC2])
        fw.op(dve, lambda e: e.scalar_tensor_tensor(out=c1, in0=zT[:, 2:2050], scalar=convw[:, cc, 2:3], in1=c2, op0=ALU.mult, op1=ALU.add), reads=[RZ, RC, RC2], writes=[RC1])
        fw.op(dve, lambda e: e.tensor_tensor(out=ucT[:, cc, :], in0=c1, in1=bgs, op=ALU.mult), reads=[RC1, RBG], writes=[RUC])
    fw.barrier()
    fw.release(m_tmp)

    mergedT = xnT_oth; RMG = Reg("merged")
    wpa_sb = fw.alloc([4, 1024], BF16); wpb_sb = fw.alloc([4, 1024], BF16); RWP = _regs(8, "wp")
    for c4 in range(4):
        fw.dma(pool, lambda e: e.dma_start(out=wpa_sb[:, c4, :], in_=wpa_d[c4]), writes=[RWP[c4]])
        fw.dma(pool, lambda e: e.dma_start(out=wpb_sb[:, c4, :], in_=wpb_d[c4]), writes=[RWP[4 + c4]])
    wgb = [fw.alloc([8, 256], BF16) for _ in range(2)]; RWGb = [_regs(8, "wg%d" % i) for i in range(2)]

    def load_wg(dc):
        for kc in range(8):
            fw.dma(pool, lambda e: e.dma_start(out=wgb[dc % 2][:, kc, :], in_=wG_d[dc, kc]), writes=[RWGb[dc % 2][kc]])

    sgAb = [fw.alloc([512], F32) for _ in range(2)]; sgBb = [fw.alloc([512], F32) for _ in range(2)]; RSGb = [_regs(2, "sg%d" % i) for i in range(2)]
    m1b = [fw.alloc([512], F32) for _ in range(2)]; m2b = [fw.alloc([512], F32) for _ in range(2)]; RMb = [_regs(2, "m%d" % i) for i in range(2)]
    load_wg(0)
    for dc in range(8):
        wg = wgb[dc % 2]; RWG = RWGb[dc % 2]
        if dc + 1 < 8:
            load_wg(dc + 1)
        for t in range(4):
            ba, bc = (0, 1) if t % 2 == 0 else (4, 5)
            sgA, sgB, RSG = sgAb[t % 2], sgBb[t % 2], RSGb[t % 2]
            m1, m2, RM = m1b[t % 2], m2b[t % 2], RMb[t % 2]
            for c4 in range(4):
                fw.op(pe, lambda e: e.matmul(out=ps[ba][:, 0:512], lhsT=wpa_sb[:, c4, dc * 128:(dc + 1) * 128], rhs=attnT[:, c4, t * 512:(t + 1) * 512], start=(c4 == 0), stop=(c4 == 3)), reads=[RWP[c4], RAT], writes=[PS[ba]])
            for c4 in range(4):
                fw.op(pe, lambda e: e.matmul(out=ps[bc][:, 0:512], lhsT=wpb_sb[:, c4, dc * 128:(dc + 1) * 128], rhs=ucT[:, c4, t * 512:(t + 1) * 512], start=(c4 == 0), stop=(c4 == 3)), reads=[RWP[4 + c4], RUC], writes=[PS[bc]])
            for gi in range(2):
                for kc in range(8):
                    fw.op(pe, lambda e: e.matmul(out=ps[2 + gi][:, 0:512], lhsT=wg[:, kc, gi * 128:(gi + 1) * 128], rhs=xt_tile(t, kc), start=(kc == 0), stop=(kc == 7)), reads=[RWG[kc]] + xt_regs(t), writes=[PS[2 + gi]])
            fw.op(act, lambda e: e.activation(out=sgA, in_=ps[2][:, 0:512], func=AF.Sigmoid), reads=[PS[2]], writes=[RSG[0]])
            fw.op(act, lambda e: e.activation(out=sgB, in_=ps[3][:, 0:512], func=AF.Sigmoid), reads=[PS[3]], writes=[RSG[1]])
            fw.op(dve, lambda e: e.tensor_tensor(out=m1, in0=ps[ba][:, 0:512], in1=sgA, op=ALU.mult), reads=[PS[ba], RSG[0]], writes=[RM[0]])
            fw.op(dve, lambda e: e.tensor_tensor(out=m2, in0=ps[bc][:, 0:512], in1=sgB, op=ALU.mult), reads=[PS[bc], RSG[1]], writes=[RM[1]])
            fw.op(pool, lambda e: e.tensor_tensor(out=mergedT[:, dc, t * 512:(t + 1) * 512], in0=m1, in1=m2, op=ALU.add), reads=[RM[0], RM[1]], writes=[RMG])
    fw.barrier()
    fw.release(m_tmp)

    hT = fw.alloc_at(off_own, [16, 1024], F32); RH = _regs(16, "h")
    wo_sb = fw.alloc([8, 1024], BF16); RWO = _regs(8, "wo")
    for dc in range(8):
        fw.dma(pool, lambda e: e.dma_start(out=wo_sb[:, dc, :], in_=wo_d[dc]), writes=[RWO[dc]])
    xres = [fw.alloc([1024], F32) for _ in range(2)]; RXR = _regs(2, "xres")
    for blk in range(16):
        dma_in(xres[blk % 2], xs_d[blk * 128:(blk + 1) * 128, :], RXR[blk % 2])
        for half in range(2):
            for dc in range(8):
                fw.op(pe, lambda e: e.matmul(out=ps[half][:, 0:512], lhsT=mergedT[:, dc, blk * 128:(blk + 1) * 128], rhs=wo_sb[:, dc, half * 512:(half + 1) * 512], start=(dc == 0), stop=(dc == 7)), reads=[RMG, RWO[dc]], writes=[PS[half]])
            fw.op(dve, lambda e: e.tensor_tensor(out=hT[:, blk, half * 512:(half + 1) * 512], in0=ps[half][:, 0:512], in1=xres[blk % 2][:, half * 512:(half + 1) * 512], op=ALU.add), reads=[PS[half], RXR[blk % 2]], writes=[RH[blk]])
    fw.barrier()
    fw.release(m_tmp)
    if dbg:
        for blk in range(16):
            outs.append(fw.dma(sp, lambda e: e.dma_start(out=dbg_d["h"][:, blk, :], in_=hT[:, blk, :]), reads=[RH[blk]]))

    wq_sb = fw.alloc_at(off_oth, [8, 2048], BF16); RWQs = _regs(16, "wq")
    rstd_all = fw.alloc([16], F32); ss_all = fw.alloc([16], F32); RRS = Reg("rstd_all")
    NR = 4
    eidx_r = [fw.alloc([128], I32) for _ in range(NR)]; REA = _regs(NR, "eidx_r")
    g_r = [fw.alloc([128], F32) for _ in range(NR)]; RGA = _regs(NR, "g_r")
    iota256 = fw.alloc([64], F32)
    fnw_rep = fw.alloc([1024], F32); finw_rep = fw.alloc([1024], F32)
    scs = fw.alloc([2048], F32); RSC = Reg("scs")
    sk_f = scs.rearrange("p (a b) -> p a b", a=16, b=128); sk_b = fw.alloc([16, 128], BF16); RSK = Reg("sk")
    dma_in(iota256, iota_d[:, 0:64], RC); dma_in(fnw_rep, fnw_rep_d, RC); dma_in(finw_rep, finw_rep_d, RC)
    dma_in(sk_f, sk_d, RSK)
    fw.barrier()
    for kc in range(8):
        for hf in range(2):
            fw.dma(pool, lambda e: e.dma_start(out=wq_sb[:, kc, hf * 1024:(hf + 1) * 1024], in_=wq_d[kc][:, hf * 1024:(hf + 1) * 1024]), writes=[RWQs[kc * 2 + hf]])
    fw.op(dve, lambda e: e.tensor_copy(out=sk_b, in_=sk_f), reads=[RSK], writes=[RSK, RSC])
    junkn = fw.alloc([1024], BF16); RJN = Reg("junkn")
    for blk in range(16):
        fw.op(act, lambda e: e.activation(out=junkn, in_=hT[:, blk, :], func=AF.Square, accum_out=ss_all[:, blk:blk + 1]), reads=[RH[blk]], writes=[RJN, RRS])
    fw.op(dve, lambda e: e.tensor_scalar(out=ss_all, in0=ss_all, scalar1=1.0 / 1024, scalar2=EPS, op0=ALU.mult, op1=ALU.add), reads=[RRS], writes=[RRS])
    fw.op(act, lambda e: e.activation(out=ss_all, in_=ss_all, func=AF.Ln), reads=[RRS], writes=[RRS])
    fw.op(act, lambda e: e.activation(out=rstd_all, in_=ss_all, func=AF.Exp, scale=-0.5), reads=[RRS], writes=[RRS])

    if P4STOP[0] == 1:
        fw.barrier(); fw.finish(outs); return nc
    RECTS = [(0, 2, 16, 0), (2, 2, 5, 32), (4, 4, 3, 42), (8, 8, 1, 54)]
    NCAND = 62
    qps = fw.alloc([2048], BF16); RQP = Reg("qps")
    top = fw.alloc([256], F32); RTOPs = [Reg("top%d" % i, disjoint=True) for i in range(16)]
    idxu = fw.alloc([256], U32); RIDXs = [Reg("idxu%d" % i, disjoint=True) for i in range(16)]
    workb = [fw.alloc([128], F32) for _ in range(2)]; RWKb = _regs(2, "workb")
    HS = [dict(cand=fw.alloc([NCAND], F32), cidx=fw.alloc([NCAND], F32), work2=fw.alloc([NCAND], F32),
               posu=fw.alloc([16], U32), posf=fw.alloc([16], F32),
               RCD=Reg("cand", disjoint=True), RCI=Reg("cidx", disjoint=True), RWK2=Reg("work2"),
               RPOS=Reg("posu", disjoint=True), RPOSF=Reg("posf")) for _ in range(2)]
    RCTs = [Reg("ctop%d" % i, disjoint=True) for i in range(8)]
    idxf = fw.alloc([256], F32); idxf128 = fw.alloc([256], F32); RIF = Reg("idxf")
    ctop = fw.alloc([128], F32); RCT = Reg("ctop", disjoint=True)
    NJ = 4
    junkc = [fw.alloc([NCAND], F32) for _ in range(NJ)]; RJ2 = _regs(NJ, "junkc")
    eidxf = fw.alloc([128], F32); REI = Reg("eidxf", disjoint=True)
    posu = fw.alloc([16], U32); RPOS = Reg("posu", disjoint=True); posf = fw.alloc([16], F32); RPOSF = Reg("posf")
    negmax = fw.alloc([8], F32); gs = fw.alloc([8], F32); rg = fw.alloc([8], F32); RSM = Reg("sm", disjoint=True)
    xnb = [fw.alloc([1024], BF16) for _ in range(1)]; RXB = _regs(1, "xnb")
    hnb = [fw.alloc([8, 128], BF16) for _ in range(2)]; RHB = [_regs(8, "hnb%d" % i) for i in range(2)]
    jc = [0]

    def route_pre(blk):
        xn = xnb[0]; rxn = RXB[0]
        hb = hnb[blk % 2]; rhb = RHB[blk % 2]
        fw.op(act, lambda e: e.activation(out=xn, in_=hT[:, blk, :], func=AF.Copy, scale=rstd_all[:, blk:blk + 1]), reads=[RH[blk], RRS], writes=[rxn])
        p = pt[blk % 2]; rp = PT[blk % 2]
        for kc in range(8):
            fw.op(pe, lambda e: e.transpose(out=p[:, kc * 128:(kc + 1) * 128], in_=xn[:, kc * 128:(kc + 1) * 128], identity=ident), reads=[rxn, RCI], writes=[rp])
        for kc in range(8):
            fw.op(act, lambda e: e.activation(out=hb[:, kc, :], in_=p[:, kc * 128:(kc + 1) * 128], func=AF.Copy, scale=fnw[:, kc:kc + 1]), reads=[rp, RC], writes=[rhb[kc]])

    def route(blk):
        slot = blk % NR
        gt = g_r[slot]; RG = RGA[slot]
        hb = hnb[blk % 2]; rhb = RHB[blk % 2]
        for g in range(4):
            bank = ps[4 + g % 2]; rb = PS[4 + g % 2]
            for j in range(4):
                hp = g * 4 + j
                for kc in range(8):
                    fw.op(pe, lambda e: e.matmul(out=bank[:, j * 128:(j + 1) * 128], lhsT=wq_sb[:, kc, hp * 128:(hp + 1) * 128], rhs=hb[:, kc, :], start=(kc == 0), stop=(kc == 7)), reads=[RWQs[kc * 2 + hp // 8], rhb[kc]], writes=[rb])
            fw.op(act, lambda e: e.activation(out=qps[:, g * 512:(g + 1) * 512], in_=bank[:, 0:512], func=AF.Copy), reads=[rb], writes=[RQP])
        yield
        for g in range(4):
            bank = ps[4 + g % 2]; rb = PS[4 + g % 2]
            for j in range(4):
                hp = g * 4 + j
                fw.op(pe, lambda e: e.matmul(out=bank[:, j * 128:(j + 1) * 128], lhsT=qps[:, hp * 128:(hp + 1) * 128], rhs=sk_b[:, hp, :], start=True, stop=True), reads=[RQP, RSK], writes=[rb])
            fw.op(act, lambda e: e.activation(out=scs[:, g * 512:(g + 1) * 512], in_=bank[:, 0:512], func=AF.Copy), reads=[rb], writes=[RSC])
        yield
        def topk_chain(hp):
            sv = scs[:, hp * 128:(hp + 1) * 128]
            wk = workb[hp % 2]; rwk = RWKb[hp % 2]
            t8a = top[:, hp * 16:hp * 16 + 8]; t8b = top[:, hp * 16 + 8:hp * 16 + 16]
            i8a = idxu[:, hp * 16:hp * 16 + 8]; i8b = idxu[:, hp * 16 + 8:hp * 16 + 16]
            return [
                lambda: fw.op(dve, lambda e: e.max(out=t8a, in_=sv), reads=[RSC], writes=[RTOPs[hp]]),
                lambda: fw.op(dve, lambda e: e.max_index(out=i8a, in_max=t8a, in_values=sv), reads=[RSC, RTOPs[hp]], writes=[RIDXs[hp]]),
                lambda: fw.op(dve, lambda e: e.match_replace(out=wk, in_to_replace=t8a, in_values=sv, imm_value=-1e30), reads=[RSC, RTOPs[hp]], writes=[rwk]),
                lambda: fw.op(dve, lambda e: e.max(out=t8b, in_=wk), reads=[rwk], writes=[RTOPs[hp]]),
                lambda: fw.op(dve, lambda e: e.max_index(out=i8b, in_max=t8b, in_values=wk), reads=[rwk, RTOPs[hp]], writes=[RIDXs[hp]]),
            ]

        for hp in range(0, 16, 2):
            ca, cb = topk_chain(hp), topk_chain(hp + 1)
            for ta, tb in zip(ca, cb):
                ta(); yield
                tb(); yield
        fw.op(dve, lambda e: e.tensor_copy(out=idxf, in_=idxu), reads=RIDXs, writes=[RIF])
        yield
        fw.op(dve, lambda e: e.tensor_scalar(out=idxf128, in0=idxf, scalar1=128.0, scalar2=None, op0=ALU.mult), reads=[RIF], writes=[RIF])
        yield

        def head_chain(hh):
            S = HS[hh % 2]
            cand_h, cidx_h, work_h, posu_h, posf_h = S["cand"], S["cidx"], S["work2"], S["posu"], S["posf"]
            rcd, rci, rwk2, rpos, rposf = S["RCD"], S["RCI"], S["RWK2"], S["RPOS"], S["RPOSF"]
            a0 = (2 * hh) * 16; b0 = (2 * hh + 1) * 16
            c8a = ctop[:, hh * 16:hh * 16 + 8]; c8b = ctop[:, hh * 16 + 8:hh * 16 + 16]
            ops = []
            for (aa0, na, nb, off) in RECTS:
                def mk(aa0=aa0, na=na, nb=nb, off=off):
                    cv = cand_h[:, off:off + na * nb].rearrange("p (a b) -> p a b", a=na, b=nb)
                    iv = cidx_h[:, off:off + na * nb].rearrange("p (a b) -> p a b", a=na, b=nb)
                    return [
                        lambda: fw.op(dve, lambda e: e.tensor_tensor(out=cv, in0=top[:, a0 + aa0:a0 + aa0 + na].unsqueeze(2).to_broadcast([128, na, nb]), in1=top[:, b0:b0 + nb].unsqueeze(1).to_broadcast([128, na, nb]), op=ALU.add), reads=[RTOPs[2 * hh], RTOPs[2 * hh + 1]], writes=[rcd]),
                        lambda: fw.op(dve, lambda e: e.tensor_tensor(out=iv, in0=idxf128[:, a0 + aa0:a0 + aa0 + na].unsqueeze(2).to_broadcast([128, na, nb]), in1=idxf[:, b0:b0 + nb].unsqueeze(1).to_broadcast([128, na, nb]), op=ALU.add), reads=[RIF], writes=[rci]),
                    ]
                ops += mk()
            ops += [
                lambda: fw.op(dve, lambda e: e.max(out=c8a, in_=cand_h), reads=[rcd], writes=[RCTs[hh]]),
                lambda: fw.op(dve, lambda e: e.match_replace(out=work_h, in_to_replace=c8a, in_values=cand_h, imm_value=-1e30), reads=[rcd, RCTs[hh]], writes=[rwk2]),
                lambda: fw.op(dve, lambda e: e.max(out=c8b, in_=work_h), reads=[rwk2], writes=[RCTs[hh]]),
                lambda: fw.op(dve, lambda e: e.max_index(out=posu_h[:, 0:8], in_max=c8a, in_values=cand_h), reads=[rcd, RCTs[hh]], writes=[rpos]),
                lambda: fw.op(dve, lambda e: e.max_index(out=posu_h[:, 8:16], in_max=c8b, in_values=work_h), reads=[rwk2, RCTs[hh]], writes=[rpos]),
                lambda: fw.op(dve, lambda e: e.tensor_copy(out=posf_h, in_=posu_h), reads=[rpos], writes=[rposf]),
            ]
            for k in range(16):
                def mk2(k=k):
                    def f():
                        jj = jc[0] % NJ; jc[0] += 1
                        fw.op(dve, lambda e: e.scalar_tensor_tensor(out=junkc[jj], in0=iota256[:, 0:NCAND], scalar=posf_h[:, k:k + 1], in1=cidx_h, op0=ALU.is_equal, op1=ALU.mult, accum_out=eidxf[:, hh * 16 + k:hh * 16 + k + 1]), reads=[RC, rposf, rci], writes=[RJ2[jj], REI])
                    return f
                ops.append(mk2())
            return ops

        for hh in range(0, 8, 2):
            ca, cb = head_chain(hh), head_chain(hh + 1)
            for ta, tb in zip(ca, cb):
                ta(); yield
                tb(); yield
        fw.op(dve, lambda e: e.tensor_scalar(out=eidxf, in0=eidxf, scalar1=16383.0, scalar2=0.0, op0=ALU.min, op1=ALU.max), reads=[REI], writes=[REI])
        yield
        fw.op(dve, lambda e: e.tensor_copy(out=eidx_r[slot], in_=eidxf), reads=[REI], writes=[REA[slot]])
        yield
        for hh in range(8):
            fw.op(dve, lambda e: e.tensor_scalar(out=negmax[:, hh:hh + 1], in0=ctop[:, hh * 16:hh * 16 + 1], scalar1=-1.0, scalar2=None, op0=ALU.mult), reads=[RCTs[hh]], writes=[RSM])
            yield
        for hh in range(8):
            fw.op(act, lambda e: e.activation(out=gt[:, hh * 16:(hh + 1) * 16], in_=ctop[:, hh * 16:(hh + 1) * 16], func=AF.Exp, bias=negmax[:, hh:hh + 1], accum_out=gs[:, hh:hh + 1]), reads=[RCTs[hh], RSM], writes=[RG, RSM])
        fw.op(dve, lambda e: e.reciprocal(out=rg, in_=gs), reads=[RSM], writes=[RSM])
        yield
        for hh in range(8):
            fw.op(dve, lambda e: e.tensor_scalar(out=gt[:, hh * 16:(hh + 1) * 16], in0=gt[:, hh * 16:(hh + 1) * 16], scalar1=rg[:, hh:hh + 1], scalar2=None, op0=ALU.mult), reads=[RG, RSM], writes=[RG])
            yield

    NG = 10
    uvb = [fw.alloc([2048], BF16) for _ in range(NG)]; RUV = _regs(NG, "uvb")
    ND = 4
    diag = [fw.alloc([128], BF16) for _ in range(ND)]; RDG = _regs(ND, "diag")
    acol = [fw.alloc([4], F32) for _ in range(ND)]; RAC = _regs(ND, "acol")
    hn_tok = [fw.alloc([1024], BF16) for _ in range(1)]; RHT = _regs(1, "hn_tok")
    NJB = 2
    junkb = [fw.alloc([1024], BF16) for _ in range(NJB)]; RJ1 = _regs(NJB, "junkb")
    acc = fw.alloc([1024], F32); RACC = Reg("acc")
    obuf = [fw.alloc([1024], F32) for _ in range(2)]; ROB = _regs(2, "obuf")
    st4 = fw.alloc([4], F32); RS4 = Reg("st4")
    gi = [0]

    def gather(blk):
        tk = slice(blk * 128, (blk + 1) * 128)
        slot = blk % NR
        ht = hn_tok[0]; rht = RHT[0]
        fw.op(dve, lambda e: e.scalar_tensor_tensor(out=ht, in0=hT[:, blk, :], scalar=rstd_all[:, blk:blk + 1], in1=fnw_rep, op0=ALU.mult, op1=ALU.mult), reads=[RH[blk], RRS, RC], writes=[rht])
        pa = (ps[0], ps[1]) if blk % 2 == 0 else (ps[2], ps[3])
        rpa = (PS[0], PS[1]) if blk % 2 == 0 else (PS[2], PS[3])
        for hk in range(128):
            b = gi[0] % NG; d = gi[0] % ND; jb = gi[0] % NJB; gi[0] += 1
            fw.dma(pool, lambda e: e.indirect_dma_start(out=uvb[b], out_offset=None, in_=uvs_d, in_offset=bass.IndirectOffsetOnAxis(ap=eidx_r[slot][:, hk:hk + 1], axis=0)), reads=[REA[slot]], writes=[RUV[b]])
            fw.op(dve, lambda e: e.scalar_tensor_tensor(out=junkb[jb], in0=uvb[b][:, 0:1024], scalar=1.0, in1=ht, op0=ALU.mult, op1=ALU.mult, accum_out=acol[d][:, 0:1]), reads=[RUV[b], rht], writes=[RJ1[jb], RAC[d]])
            fw.op(act, lambda e: e.activation(out=acol[d][:, 1:2], in_=acol[d][:, 0:1], func=AF.Gelu), reads=[RAC[d]], writes=[RAC[d]])
            fw.op(act, lambda e: e.activation(out=acol[d][:, 2:3], in_=acol[d][:, 1:2], func=AF.Copy, scale=g_r[slot][:, hk:hk + 1]), reads=[RAC[d], RGA[slot]], writes=[RAC[d]])
            fw.op(act, lambda e: e.activation(out=diag[d], in_=ident, func=AF.Copy, scale=acol[d][:, 2:3]), reads=[RCI, RAC[d]], writes=[RDG[d]])
            for half in range(2):
                fw.op(pe, lambda e: e.matmul(out=pa[half][:, 0:512], lhsT=diag[d], rhs=uvb[b][:, 1024 + half * 512:1024 + (half + 1) * 512], start=(hk == 0), stop=(hk == 127)), reads=[RDG[d], RUV[b]], writes=[rpa[half]])
            yield
        for half in range(2):
            fw.op(dve, lambda e: e.tensor_tensor(out=acc[:, half * 512:(half + 1) * 512], in0=pa[half][:, 0:512], in1=hT[:, blk, half * 512:(half + 1) * 512], op=ALU.add), reads=[rpa[half], RH[blk]], writes=[RACC])
        ob = obuf[blk % 2]; rob = ROB[blk % 2]
        rms_rstd(acc, 1024, st4, RS4, ob, rob, [RACC])
        fw.op(dve, lambda e: e.scalar_tensor_tensor(out=ob, in0=acc, scalar=st4[:, 2:3], in1=finw_rep, op0=ALU.mult, op1=ALU.mult), reads=[RACC, RS4, RC], writes=[rob])
        outs.append(fw.dma(sp, lambda e: e.dma_start(out=out_d[tk, :], in_=ob), reads=[rob]))

    RSTEPS = RSTEPS_CFG[0]
    if P4STOP[0] == 5:
        def load_h(blk, xs, rx):
            return hT[:, blk, :], [RH[blk]]
        RTMP = _regs(1, "tmpdst")
        norm_transpose(load_h, 1, lambda blk, kc: hnb[0][:, kc, :], fnw, RTMP)
        fw.barrier(); fw.finish(outs); return nc
    if P4STOP[0] == 6:
        xn = xnb[0]
        fw.op(act, lambda e: e.activation(out=xn, in_=hT[:, 0, :], func=AF.Copy, scale=rstd_all[:, 0:1]), reads=[RH[0], RRS], writes=[RXB[0]])
        fw.barrier(); fw.finish(outs); return nc
    if P4STOP[0] in (7, 8, 9):
        xn = xnb[0]
        fw.op(act, lambda e: e.activation(out=xn, in_=hT[:, 0, :], func=AF.Copy, scale=rstd_all[:, 0:1]), reads=[RH[0], RRS], writes=[RXB[0]])
        for kc in range(8):
            fw.op(pe, lambda e: e.transpose(out=pt[0][:, kc * 128:(kc + 1) * 128], in_=xn[:, kc * 128:(kc + 1) * 128], identity=ident), reads=[RXB[0], RCI], writes=[PT[0]])
        if P4STOP[0] == 8:
            for kc in range(0, 8, 2):
                fw.op(dve, lambda e: e.tensor_scalar(out=hnb[0][:, kc, :], in0=pt[0][:, kc * 128:(kc + 1) * 128], scalar1=fnw[:, kc:kc + 1], scalar2=None, op0=ALU.mult), reads=[PT[0], RC], writes=[RHB[0][kc]])
        if P4STOP[0] == 9:
            for kc in range(1, 8, 2):
                fw.op(act, lambda e: e.activation(out=hnb[0][:, kc, :], in_=pt[0][:, kc * 128:(kc + 1) * 128], func=AF.Copy, scale=fnw[:, kc:kc + 1]), reads=[PT[0], RC], writes=[RHB[0][kc]])
        fw.barrier(); fw.finish(outs); return nc
    route_pre(0)
    if P4STOP[0] == 2:
        fw.barrier(); fw.finish(outs); return nc
    for _ in route(0):
        pass
    if P4STOP[0] == 3:
        fw.barrier(); fw.finish(outs); return nc
    if P4STOP[0] == 4:
        for _ in gather(0):
            pass
        fw.barrier(); fw.finish(outs); return nc
    for blk in range(16):
        if blk + 1 < 16:
            route_pre(blk + 1)
        rr = route(blk + 1) if blk + 1 < 16 else None
        for _ in gather(blk):
            if rr is not None:
                for _i in range(RSTEPS):
                    try:
                        next(rr)
                    except StopIteration:
                        rr = None
                        break
        if rr is not None:
            for _ in rr:
                pass
    fw.finish(outs)
    return nc


_CACHE = {}


def _prep_shared(inp):
    f = np.float32
    w_in = np.asarray(inp["w_in"], f)[0]
    perm = np.array([m * 64 + (d + 32) % 64 for m in range(2) for d in range(64)])
    wA = []
    for h in range(4):
        q = w_in[:, h * 128:(h + 1) * 128]
        k = w_in[:, 512 + h * 128:512 + (h + 1) * 128]
        v = w_in[:, 1024 + h * 128:1024 + (h + 1) * 128]
        wA.append(np.concatenate([q, q[:, perm], k, k[:, perm], v], axis=1).reshape(8, 128, 640))
    wA = np.ascontiguousarray(np.stack(wA))
    bg = w_in[:, 1536:2048]; cg = w_in[:, 2048:2560]; xc = w_in[:, 2560:3072]
    ga = w_in[:, 3072:4096]; gb = w_in[:, 4096:5120]
    wC = np.ascontiguousarray(np.stack([np.concatenate([bg[:, c * 128:(c + 1) * 128], cg[:, c * 128:(c + 1) * 128], xc[:, c * 128:(c + 1) * 128]], axis=1).reshape(8, 128, 384) for c in range(4)]))
    wG = np.ascontiguousarray(np.stack([np.concatenate([ga[:, c * 128:(c + 1) * 128], gb[:, c * 128:(c + 1) * 128]], axis=1).reshape(8, 128, 256) for c in range(8)]))
    sh = dict(
        ident=np.eye(128, dtype=f),
        iota256=np.ascontiguousarray(np.broadcast_to(np.arange(256, dtype=f)[None, :], (128, 256))),
        anw=np.ascontiguousarray(np.asarray(inp["attn_norm_w"], f)[0].reshape(8, 128).T),
        fnw=np.ascontiguousarray(np.asarray(inp["ffn_norm_w"], f)[0].reshape(8, 128).T),
        fnw_rep=np.ascontiguousarray(np.broadcast_to(np.asarray(inp["ffn_norm_w"], f)[0][None, :], (128, 1024))),
        finw_rep=np.ascontiguousarray(np.broadcast_to(np.asarray(inp["final_norm_w"], f)[None, :], (128, 1024))),
        subln_rep=np.ascontiguousarray(np.broadcast_to(np.asarray(inp["subln_w"], f)[0][None, :], (128, 128))),
        subcol=np.ascontiguousarray(np.asarray(inp["subln_w"], f)[0].reshape(128, 1)),
        lam_in=np.ascontiguousarray(np.broadcast_to(np.stack([np.asarray(inp[k], f)[0] for k in ("lambda_q1", "lambda_k1", "lambda_q2", "lambda_k2")])[None], (128, 4, 64))),
        convw=np.ascontiguousarray(np.asarray(inp["conv_w"], f)[0].reshape(3, 4, 128).transpose(2, 1, 0)),
        wA=wA, wC=wC, wG=wG,
        wpa=np.ascontiguousarray(np.asarray(inp["w_proj_attn"], f)[0].reshape(4, 128, 1024)),
        wpb=np.ascontiguousarray(np.asarray(inp["w_proj_conv"], f)[0].reshape(4, 128, 1024)),
        wo=np.ascontiguousarray(np.asarray(inp["w_out"], f)[0].reshape(8, 128, 1024)),
        wq=np.ascontiguousarray(np.asarray(inp["w_query"], f)[0].reshape(8, 128, 2048)),
        skT=np.ascontiguousarray(np.asarray(inp["sub_keys"], f)[0].reshape(16, 128, 128).transpose(2, 0, 1)),
        expert_u=np.ascontiguousarray(np.asarray(inp["expert_u"], f)[0]),
        expert_v=np.ascontiguousarray(np.asarray(inp["expert_v"], f)[0]),
    )
    return sh


def _rope_tables():
    inv_freq = (1.0 / (10000.0 ** (np.arange(0, 64, 2, dtype=np.float32) / np.float32(64)))).astype(np.float32)
    pos = np.arange(SEQ, dtype=np.float32)
    ang = (pos[:, None] * inv_freq[None, :]).astype(np.float32)
    d = np.arange(128) % 64
    cos = np.cos(ang)[:, d % 32].T.astype(np.float32)
    sin = np.sin(ang)[:, d % 32].T.astype(np.float32)
    sign = np.where(d < 32, -1.0, 1.0).astype(np.float32)[:, None]
    return cos, (sin * sign).astype(np.float32)


def _core_inputs(inp, sh, cos, sin, c):
    b, half = c // 2, c % 2
    x = np.asarray(inp["x"], np.float32)
    own = slice(half * T_OWN, (half + 1) * T_OWN)
    oth = slice((1 - half) * T_OWN, (2 - half) * T_OWN)
    m = dict(sh)
    m["xs"] = np.ascontiguousarray(np.concatenate([x[b, own], x[b, oth]], axis=0))
    m["cosT"] = np.ascontiguousarray(np.concatenate([cos[:, own], cos[:, oth]], axis=1))
    m["sinT"] = np.ascontiguousarray(np.concatenate([sin[:, own], sin[:, oth]], axis=1))
    fl = np.zeros((128, 2), np.float32)
    fl[:, 0] = 1.0 if half == 1 else 0.0
    fl[:, 1] = 1.0 if half == 0 else 0.0
    m["flags"] = fl
    return m


def kernel(**inputs):
    if "nc" not in _CACHE:
        _CACHE["nc"] = build_program(dbg=False)
    nc = _CACHE["nc"]
    sh = _prep_shared(inputs)
    cos, sin = _rope_tables()
    in_maps = [_core_inputs(inputs, sh, cos, sin, c) for c in range(8)]
    res = run_bass_kernel_spmd(nc, in_maps, core_ids=list(range(8)))
    out = np.empty((NB, SEQ, D_MODEL), np.float32)
    for c in range(8):
        b, half = c // 2, c % 2
        out[b, half * T_OWN:(half + 1) * T_OWN] = np.asarray(res.results[c]["out"], np.float32)
    return out
```
